# Optimizing a Trainium2 kernel written in Bass

```python
import math
import jax, jax.numpy as jnp
from jax import lax
import numpy as np

D_MODEL = 4096
BATCH = 1
SEQ = 8192
DEPTH = 2

GRID_W = 64
CTX_LEN = 256
HEAD_DIM = 128
GQA_HEADS = D_MODEL // 256
GQA_KV_HEADS = GQA_HEADS // 4
DIFF_HEADS = D_MODEL // 512
DIFF_QK_DIM = HEAD_DIM
DIFF_V_DIM = 2 * HEAD_DIM
GQA_Q_W = GQA_HEADS * HEAD_DIM
GQA_KV_W = GQA_KV_HEADS * HEAD_DIM
DIFF_QK_W = DIFF_HEADS * 2 * DIFF_QK_DIM
DIFF_V_W = DIFF_HEADS * DIFF_V_DIM
ATTN_SPLITS = (GQA_Q_W, GQA_Q_W + GQA_KV_W, GQA_Q_W + 2 * GQA_KV_W,
               GQA_Q_W + 2 * GQA_KV_W + DIFF_QK_W, GQA_Q_W + 2 * GQA_KV_W + 2 * DIFF_QK_W)
ATTN_IN_W = ATTN_SPLITS[-1] + DIFF_V_W
ATTN_OUT_W = GQA_Q_W + DIFF_V_W
Q_BLOCK = 128
ROPE_THETA = 10000.0
D_FF = 256 * math.ceil(8 * D_MODEL / 3 / 256)
D_RNN = D_MODEL
LRU_BLOCKS = D_MODEL // 256
LRU_BLOCK_W = D_RNN // LRU_BLOCKS
CONV_W = 4
CONV_LEFT = CONV_W // 2
RG_C = 8.0
N_EXPERTS = 8
TOP_K = 2
EXPERT_FF = D_MODEL
N_ATTN_LAYERS = (DEPTH + 1) // 2
N_REC_LAYERS = DEPTH // 2
EPS = 1e-6
F32 = jnp.float32

kernel_name = 'hybrid_diffusion_gqa_diffattn_rglru_moe'


def rms_norm(x, g):
    xf = x.astype(F32)
    y = xf * lax.rsqrt(jnp.mean(xf * xf, axis=-1, keepdims=True) + EPS)
    return (y * g.astype(F32)).astype(x.dtype)


def modulate(x, g, shift, scale):
    return rms_norm(x, g) * (1.0 + scale) + shift


def adaln(cvec, w_mod, b_mod):
    m = (jax.nn.silu(cvec) @ w_mod + b_mod).reshape(-1, 1, 6, D_MODEL)
    return tuple(m[:, :, j] for j in range(6))


def axial_rope(rows, head_dim):
    half = head_dim // 2
    inv = ROPE_THETA ** (-jnp.arange(0, half, 2, dtype=F32) / half)
    row = jnp.repeat(jnp.arange(rows, dtype=F32), GRID_W)
    col = jnp.tile(jnp.arange(GRID_W, dtype=F32), rows)
    ang = jnp.concatenate([row[:, None] * inv, col[:, None] * inv], axis=-1)
    return jnp.cos(ang), jnp.sin(ang)


def apply_rope(x, cos, sin):
    shape = (1, x.shape[1]) + (1,) * (x.ndim - 3) + (cos.shape[-1],)
    c = cos.reshape(shape)
    s = sin.reshape(shape)
    xf = x.astype(F32)
    x1, x2 = xf[..., 0::2], xf[..., 1::2]
    return jnp.stack([x1 * c - x2 * s, x1 * s + x2 * c], axis=-1).reshape(x.shape).astype(x.dtype)


def sweep_query_blocks(fn, q, *kv):
    b, s = q.shape[:2]
    nb = s // Q_BLOCK
    qb = jnp.moveaxis(q.reshape((b, nb, Q_BLOCK) + q.shape[2:]), 1, 0)
    out = lax.map(lambda blk: fn(blk, *kv), qb)
    return jnp.moveaxis(out, 0, 1).reshape((b, s) + out.shape[3:])


def gqa_attend(q, k, v):
    b, t, h, d = q.shape
    hkv = k.shape[2]
    qg = q.reshape(b, t, hkv, h // hkv, d)
    s = jnp.einsum('bqhgd,bkhd->bhgqk', qg, k).astype(F32) * d ** -0.5
    p = jax.nn.softmax(s, axis=-1).astype(v.dtype)
    return jnp.einsum('bhgqk,bkhd->bqhgd', p, v).reshape(b, t, h, d)


def diff_attend(q, k, v, lam):
    d = q.shape[-1]
    s = jnp.einsum('bqhmd,bkhmd->bhmqk', q, k).astype(F32) * d ** -0.5
    p = jax.nn.softmax(s, axis=-1)
    a = p[:, :, 0] - lam * p[:, :, 1]
    return jnp.einsum('bhqk,bkhe->bqhe', a.astype(v.dtype), v)


def swiglu(h, w_gate, w_up, w_down):
    return (jax.nn.silu(h @ w_gate) * (h @ w_up)) @ w_down


def _attn_heads(h, w_in, gqa_qn, gqa_kn, diff_qn, diff_kn):
    b, t, _ = h.shape
    q_a, k_a, v_a, q_b, k_b, v_b = jnp.split(h @ w_in, ATTN_SPLITS, axis=-1)
    q_a = rms_norm(q_a.reshape(b, t, GQA_HEADS, HEAD_DIM), gqa_qn)
    k_a = rms_norm(k_a.reshape(b, t, GQA_KV_HEADS, HEAD_DIM), gqa_kn)
    v_a = v_a.reshape(b, t, GQA_KV_HEADS, HEAD_DIM)
    q_b = rms_norm(q_b.reshape(b, t, DIFF_HEADS, 2, DIFF_QK_DIM), diff_qn)
    k_b = rms_norm(k_b.reshape(b, t, DIFF_HEADS, 2, DIFF_QK_DIM), diff_kn)
    v_b = v_b.reshape(b, t, DIFF_HEADS, DIFF_V_DIM)
    return q_a, k_a, v_a, q_b, k_b, v_b


def _attn_merge(o_a, o_b, subln, lam_init, w_out):
    b, t = o_a.shape[:2]
    o_b = rms_norm(o_b, subln) * (1.0 - lam_init)
    return jnp.concatenate([o_a.reshape(b, t, GQA_Q_W), o_b.reshape(b, t, DIFF_V_W)], axis=-1) @ w_out


def attention_mixer(h_lat, h_ctx, cos, sin, lam_init, need_ctx, w_in, gqa_qn, gqa_kn, diff_qn, diff_kn,
                    lam_q1, lam_k1, lam_q2, lam_k2, subln, w_out):
    lam = (jnp.exp(jnp.sum(lam_q1.astype(F32) * lam_k1.astype(F32)))
           - jnp.exp(jnp.sum(lam_q2.astype(F32) * lam_k2.astype(F32))) + lam_init)
    qa_l, ka_l, va_l, qb_l, kb_l, vb_l = _attn_heads(h_lat, w_in, gqa_qn, gqa_kn, diff_qn, diff_kn)
    qa_c, ka_c, va_c, qb_c, kb_c, vb_c = _attn_heads(h_ctx, w_in, gqa_qn, gqa_kn, diff_qn, diff_kn)
    qa_l = apply_rope(qa_l, cos, sin)
    ka_l = apply_rope(ka_l, cos, sin)
    qb_l = apply_rope(qb_l, cos, sin)
    kb_l = apply_rope(kb_l, cos, sin)
    ka = jnp.concatenate([ka_c, ka_l], axis=1)
    va = jnp.concatenate([va_c, va_l], axis=1)
    kb = jnp.concatenate([kb_c, kb_l], axis=1)
    vb = jnp.concatenate([vb_c, vb_l], axis=1)
    o_a = sweep_query_blocks(gqa_attend, qa_l, ka, va)
    o_b = sweep_query_blocks(lambda q, k, v: diff_attend(q, k, v, lam), qb_l, kb, vb)
    out_lat = _attn_merge(o_a, o_b, subln, lam_init, w_out)
    if not need_ctx:
        return out_lat, None
    out_ctx = _attn_merge(gqa_attend(qa_c, ka_c, va_c), diff_attend(qb_c, kb_c, vb_c, lam), subln, lam_init, w_out)
    return out_lat, out_ctx


def depthwise_conv_centred(x, w, b):
    t = x.shape[1]
    xp = jnp.pad(x, ((0, 0), (CONV_LEFT, CONV_W - 1 - CONV_LEFT), (0, 0)))
    y = xp[:, 0:t] * w[0]
    for k in range(1, CONV_W):
        y = y + xp[:, k:k + t] * w[k]
    return y + b


def block_diag_linear(x, w, b):
    xb = x.reshape(x.shape[:-1] + (LRU_BLOCKS, LRU_BLOCK_W))
    return jnp.einsum('btnd,nde->btne', xb, w).reshape(x.shape) + b


def _combine(e1, e2):
    a1, b1 = e1
    a2, b2 = e2
    return a1 * a2, a2 * b1 + b2


def linear_scan(a, b, h0, reverse):
    if reverse:
        a = jnp.flip(a, axis=1)
        b = jnp.flip(b, axis=1)
    b = b.at[:, 0].add(a[:, 0] * h0)
    _, h = lax.associative_scan(_combine, (a, b), axis=1)
    if reverse:
        h = jnp.flip(h, axis=1)
    return h


def rglru_scan(xc, w_a, b_a, w_x, b_x, lam, h0, reverse):
    r = jax.nn.sigmoid(block_diag_linear(xc, w_a, b_a).astype(F32))
    i = jax.nn.sigmoid(block_diag_linear(xc, w_x, b_x).astype(F32))
    log_a = -RG_C * r * jax.nn.softplus(-lam.astype(F32))
    a = jnp.exp(log_a)
    b = jnp.sqrt(-jnp.expm1(2.0 * log_a)) * (i * xc.astype(F32))
    return linear_scan(a, b, h0, reverse)


def recurrent_mixer(h_lat, h_ctx, need_ctx, w_in, conv_w, conv_b, ga_w, ga_b, gx_w, gx_b, lru_lam, w_out):
    y_lat, xr_lat = jnp.split(h_lat @ w_in, 2, axis=-1)
    xc_lat = depthwise_conv_centred(xr_lat, conv_w, conv_b)
    if need_ctx:
        y_ctx, xr_ctx = jnp.split(h_ctx @ w_in, 2, axis=-1)
    else:
        xr_ctx = h_ctx @ w_in[:, D_RNN:]
    xc_ctx = depthwise_conv_centred(xr_ctx, conv_w, conv_b)
    h0 = jnp.zeros((h_lat.shape[0], D_RNN), F32)
    s_lat = jnp.zeros(xc_lat.shape, F32)
    s_ctx = jnp.zeros(xc_ctx.shape, F32)
    for d, reverse in enumerate((False, True)):
        p = (ga_w[d], ga_b[d], gx_w[d], gx_b[d], lru_lam[d])
        hc = rglru_scan(xc_ctx, *p, h0, reverse)
        h_end = hc[:, 0] if reverse else hc[:, -1]
        s_lat = s_lat + rglru_scan(xc_lat, *p, h_end, reverse)
        if need_ctx:
            s_ctx = s_ctx + hc
    out_lat = (s_lat.astype(h_lat.dtype) * jax.nn.gelu(y_lat, approximate=True)) @ w_out
    if not need_ctx:
        return out_lat, None
    out_ctx = (s_ctx.astype(h_ctx.dtype) * jax.nn.gelu(y_ctx, approximate=True)) @ w_out
    return out_lat, out_ctx


def moe_swiglu(h, router_w, router_b, w_gate, w_up, w_down):
    logits = (h @ router_w).astype(F32) + router_b.astype(F32)
    top_v, top_i = lax.top_k(logits, TOP_K)
    top_w = jax.nn.softmax(top_v, axis=-1)
    comb = jnp.einsum('btk,btke->bte', top_w, jax.nn.one_hot(top_i, N_EXPERTS, dtype=F32)).astype(h.dtype)
    out = jnp.zeros_like(h)
    for e in range(N_EXPERTS):
        out = out + comb[..., e:e + 1] * swiglu(h, w_gate[e], w_up[e], w_down[e])
    return out


def setup_inputs(seed: int = 0) -> dict:
    key = jax.random.key(seed)
    keys = iter(jax.random.split(key, 64))
    na, nr, d = N_ATTN_LAYERS, N_REC_LAYERS, D_MODEL

    def normal(shape, scale):
        return scale * jax.random.normal(next(keys), shape, F32)

    def gain(shape):
        return 1.0 + 0.05 * jax.random.normal(next(keys), shape, F32)

    u = jax.random.uniform(next(keys), (nr, 2, D_RNN), F32, 0.9, 0.999)
    s = u ** (1.0 / RG_C)
    return {
        'x': normal((BATCH, SEQ, d), 1.0),
        'c': normal((BATCH, d), 1.0),
        'ctx': normal((BATCH, CTX_LEN, d), 1.0),
        'c_ctx': normal((d,), 1.0),
        'attn_w_mod': normal((na, d, 6 * d), d ** -0.5),
        'attn_b_mod': normal((na, 6 * d), 0.02),
        'attn_norm_mix': gain((na, d)),
        'attn_norm_ffn': gain((na, d)),
        'attn_w_in': normal((na, d, ATTN_IN_W), d ** -0.5),
        'attn_gqa_q_norm': gain((na, HEAD_DIM)),
        'attn_gqa_k_norm': gain((na, HEAD_DIM)),
        'attn_diff_q_norm': gain((na, 2, DIFF_QK_DIM)),
        'attn_diff_k_norm': gain((na, 2, DIFF_QK_DIM)),
        'attn_diff_lambda_q1': normal((na, DIFF_QK_DIM), 0.1),
        'attn_diff_lambda_k1': normal((na, DIFF_QK_DIM), 0.1),
        'attn_diff_lambda_q2': normal((na, DIFF_QK_DIM), 0.1),
        'attn_diff_lambda_k2': normal((na, DIFF_QK_DIM), 0.1),
        'attn_diff_subln': gain((na, DIFF_V_DIM)),
        'attn_w_out': normal((na, ATTN_OUT_W, d), ATTN_OUT_W ** -0.5),
        'ffn_w_gate': normal((na, d, D_FF), d ** -0.5),
        'ffn_w_up': normal((na, d, D_FF), d ** -0.5),
        'ffn_w_down': normal((na, D_FF, d), D_FF ** -0.5),
        'rec_w_mod': normal((nr, d, 6 * d), d ** -0.5),
        'rec_b_mod': normal((nr, 6 * d), 0.02),
        'rec_norm_mix': gain((nr, d)),
        'rec_norm_ffn': gain((nr, d)),
        'rec_w_in': normal((nr, d, 2 * D_RNN), d ** -0.5),
        'rec_conv_w': normal((nr, CONV_W, D_RNN), CONV_W ** -0.5),
        'rec_conv_b': normal((nr, D_RNN), 0.02),
        'rec_gate_a_w': normal((nr, 2, LRU_BLOCKS, LRU_BLOCK_W, LRU_BLOCK_W), LRU_BLOCK_W ** -0.5),
        'rec_gate_a_b': normal((nr, 2, D_RNN), 0.02),
        'rec_gate_x_w': normal((nr, 2, LRU_BLOCKS, LRU_BLOCK_W, LRU_BLOCK_W), LRU_BLOCK_W ** -0.5),
        'rec_gate_x_b': normal((nr, 2, D_RNN), 0.02),
        'rec_lru_lambda': jnp.log(s) - jnp.log1p(-s),
        'rec_w_out': normal((nr, D_RNN, d), D_RNN ** -0.5),
        'moe_router_w': normal((nr, d, N_EXPERTS), d ** -0.5),
        'moe_router_b': normal((nr, N_EXPERTS), 0.01),
        'moe_w_gate': normal((nr, N_EXPERTS, d, EXPERT_FF), d ** -0.5),
        'moe_w_up': normal((nr, N_EXPERTS, d, EXPERT_FF), d ** -0.5),
        'moe_w_down': normal((nr, N_EXPERTS, EXPERT_FF, d), EXPERT_FF ** -0.5),
    }


def reference(x, c, ctx, c_ctx,
              attn_w_mod, attn_b_mod, attn_norm_mix, attn_norm_ffn, attn_w_in,
              attn_gqa_q_norm, attn_gqa_k_norm, attn_diff_q_norm, attn_diff_k_norm,
              attn_diff_lambda_q1, attn_diff_lambda_k1, attn_diff_lambda_q2, attn_diff_lambda_k2,
              attn_diff_subln, attn_w_out, ffn_w_gate, ffn_w_up, ffn_w_down,
              rec_w_mod, rec_b_mod, rec_norm_mix, rec_norm_ffn, rec_w_in, rec_conv_w, rec_conv_b,
              rec_gate_a_w, rec_gate_a_b, rec_gate_x_w, rec_gate_x_b, rec_lru_lambda, rec_w_out,
              moe_router_w, moe_router_b, moe_w_gate, moe_w_up, moe_w_down):
    rows = x.shape[1] // GRID_W
    cos, sin = axial_rope(rows, HEAD_DIM)
    for layer in range(DEPTH):
        last = layer == DEPTH - 1
        i = layer // 2
        if layer % 2 == 0:
            l_sh1, l_sc1, l_g1, l_sh2, l_sc2, l_g2 = adaln(c, attn_w_mod[i], attn_b_mod[i])
            c_sh1, c_sc1, c_g1, c_sh2, c_sc2, c_g2 = adaln(c_ctx, attn_w_mod[i], attn_b_mod[i])
            lam_init = 0.8 - 0.6 * math.exp(-0.3 * layer)
            mix_lat, mix_ctx = attention_mixer(
                modulate(x, attn_norm_mix[i], l_sh1, l_sc1),
                modulate(ctx, attn_norm_mix[i], c_sh1, c_sc1),
                cos, sin, lam_init, not last, attn_w_in[i],
                attn_gqa_q_norm[i], attn_gqa_k_norm[i], attn_diff_q_norm[i], attn_diff_k_norm[i],
                attn_diff_lambda_q1[i], attn_diff_lambda_k1[i], attn_diff_lambda_q2[i], attn_diff_lambda_k2[i],
                attn_diff_subln[i], attn_w_out[i])
            x = x + l_g1 * mix_lat
            x = x + l_g2 * swiglu(modulate(x, attn_norm_ffn[i], l_sh2, l_sc2), ffn_w_gate[i], ffn_w_up[i], ffn_w_down[i])
            if not last:
                ctx = ctx + c_g1 * mix_ctx
                ctx = ctx + c_g2 * swiglu(modulate(ctx, attn_norm_ffn[i], c_sh2, c_sc2), ffn_w_gate[i], ffn_w_up[i], ffn_w_down[i])
        else:
            l_sh1, l_sc1, l_g1, l_sh2, l_sc2, l_g2 = adaln(c, rec_w_mod[i], rec_b_mod[i])
            c_sh1, c_sc1, c_g1, c_sh2, c_sc2, c_g2 = adaln(c_ctx, rec_w_mod[i], rec_b_mod[i])
            mix_lat, mix_ctx = recurrent_mixer(
                modulate(x, rec_norm_mix[i], l_sh1, l_sc1),
                modulate(ctx, rec_norm_mix[i], c_sh1, c_sc1), not last,
                rec_w_in[i], rec_conv_w[i], rec_conv_b[i], rec_gate_a_w[i], rec_gate_a_b[i],
                rec_gate_x_w[i], rec_gate_x_b[i], rec_lru_lambda[i], rec_w_out[i])
            x = x + l_g1 * mix_lat
            x = x + l_g2 * moe_swiglu(modulate(x, rec_norm_ffn[i], l_sh2, l_sc2), moe_router_w[i], moe_router_b[i],
                                      moe_w_gate[i], moe_w_up[i], moe_w_down[i])
            if not last:
                ctx = ctx + c_g1 * mix_ctx
                ctx = ctx + c_g2 * moe_swiglu(modulate(ctx, rec_norm_ffn[i], c_sh2, c_sc2), moe_router_w[i], moe_router_b[i],
                                              moe_w_gate[i], moe_w_up[i], moe_w_down[i])
    return x
```

```python
import numpy as np
from contextlib import ExitStack
import ml_dtypes
import concourse.bass as bass
import concourse.mybir as mybir
from concourse.bass_utils import run_bass_kernel_spmd

F32 = mybir.dt.float32
BF16 = mybir.dt.bfloat16
AF = mybir.ActivationFunctionType
ALU = mybir.AluOpType
AX = mybir.AxisListType
NPBF = ml_dtypes.bfloat16

NCORES = 8
D = 4096
KC = 32
S = 8192
TL = 1024
TC = 256
EPS = 1e-6
ENGINES = ['sync', 'scalar', 'vector', 'gpsimd', 'tensor']
ARENA = 102400


class Tok:
    __slots__ = ('name', 'writers', 'readers', 'pending_war')

    def __init__(self, name=''):
        self.name = name
        self.writers = []
        self.readers = []
        self.pending_war = []


class Op:
    __slots__ = ('eng', 'fn', 'deps', 'dma_sem', 'idx', 'signal', 'sem', 'val')


class Prog:
    def __init__(self, nc):
        self.nc = nc
        self.ops = []
        self.eng_ops = {e: [] for e in ENGINES}

    def op(self, eng, fn, reads=(), writes=(), dma=None, pwrites=()):
        o = Op()
        o.eng = eng
        o.fn = fn
        o.deps = set()
        o.dma_sem = dma
        o.signal = False
        o.sem = None
        o.val = 0
        o.idx = len(self.ops)
        for t in reads:
            for w in t.writers:
                o.deps.add(w)
            t.readers.append(o)
        for t in writes:
            self._write(o, t, True)
        for t in pwrites:
            self._write(o, t, False)
        o.deps.discard(o)
        self.ops.append(o)
        self.eng_ops[eng].append(o)
        return o

    def _write(self, o, t, exclusive):
        if t.readers:
            for r in t.readers:
                o.deps.add(r)
            t.pending_war = t.readers
            t.readers = []
            if exclusive:
                for w in t.writers:
                    o.deps.add(w)
            t.writers = [o]
        else:
            for r in t.pending_war:
                o.deps.add(r)
            if exclusive:
                for w in t.writers:
                    o.deps.add(w)
                t.writers = [o]
            else:
                t.writers.append(o)

    def barrier(self):
        last = []
        for e in ENGINES:
            if self.eng_ops[e]:
                for o in reversed(self.eng_ops[e]):
                    if o.fn is not None and o.dma_sem is None:
                        last.append(o)
                        break
        seen = {}
        for o in self.ops:
            if o.dma_sem is not None:
                seen[o.dma_sem] = o
        last.extend(seen.values())
        for e in ENGINES:
            b = self.op(e, None)
            for o in last:
                b.deps.add(o)

    def emit(self, stack):
        nc = self.nc
        for o in self.ops:
            for d in o.deps:
                if d.eng == 'tensor' and o.eng == 'tensor' and d.dma_sem is None:
                    continue
                d.signal = True
        eng_sem = {e: stack.enter_context(nc.semaphore('s_' + e)) for e in ENGINES}
        dsem = {}
        dcount = {}
        for o in self.ops:
            if o.dma_sem is not None and o.dma_sem not in dsem:
                dsem[o.dma_sem] = stack.enter_context(nc.semaphore('d_' + o.dma_sem))
                dcount[o.dma_sem] = 0
        ecount = {e: 0 for e in ENGINES}
        for o in self.ops:
            if o.fn is None:
                continue
            if o.dma_sem is not None:
                dcount[o.dma_sem] += 16
                o.sem = dsem[o.dma_sem]
                o.val = dcount[o.dma_sem]
                o.signal = True
            elif o.signal:
                ecount[o.eng] += 1
                o.sem = eng_sem[o.eng]
                o.val = ecount[o.eng]
        self.n_sems = len(eng_sem) + len(dsem)
        block = stack.enter_context(nc.Block())
        prog = self

        def make(ename):
            def body(eng):
                waited = {}
                for o in prog.eng_ops[ename]:
                    need = {}
                    for d in o.deps:
                        if d.fn is None:
                            continue
                        if d.eng == 'tensor' and ename == 'tensor' and d.dma_sem is None:
                            continue
                        k = id(d.sem)
                        if k not in need or need[k][1] < d.val:
                            need[k] = (d.sem, d.val)
                    for k, (s, v) in need.items():
                        if waited.get(k, 0) >= v:
                            continue
                        eng.wait_ge(s, v)
                        waited[k] = v
                    if o.fn is None:
                        continue
                    ins = o.fn(eng)
                    if o.signal:
                        ins.then_inc(o.sem, 16 if o.dma_sem is not None else 1)
                if ename == 'sync':
                    for nm, s in dsem.items():
                        eng.wait_ge(s, dcount[nm])
            return body

        block.sync(make('sync'))
        block.scalar(make('scalar'))
        block.vector(make('vector'))
        block.gpsimd(make('gpsimd'))
        block.tensor(make('tensor'))


class Pool:
    def __init__(self, kb, name, n, cols, dt):
        self.bufs = [kb.sb(cols, dt) for _ in range(n)]
        self.toks = [Tok(f"{name}{i}") for i in range(n)]
        self.i = 0
        self.name = name
        self.n = n

    def next(self):
        i = self.i % self.n
        self.i += 1
        return self.bufs[i], self.toks[i], f"{self.name}{i}"


class KB:
    def __init__(self):
        self.nc = bass.Bass("TRN2", target_bir_lowering=False)
        nc = self.nc
        self.P = Prog(nc)
        self.st = ExitStack()
        self.arena = self.st.enter_context(nc.sbuf_tensor("arena", [128, ARENA], BF16))
        self.off = 0
        self.ps = [self.st.enter_context(nc.psum_tensor(f"ps{i}", [128, 512], F32)) for i in range(8)]
        self.pst = [Tok(f"ps{i}") for i in range(8)]
        self.outs = []

    def din(self, name, shape, dt=F32):
        return self.nc.dram_tensor(name, list(shape), dt, kind="ExternalInput").ap()

    def dout(self, name, shape, dt=F32):
        self.outs.append(name)
        return self.nc.dram_tensor(name, list(shape), dt, kind="ExternalOutput").ap()

    def dscr(self, name, shape, dt=F32):
        return self.nc.dram_tensor(name, list(shape), dt, kind="Internal").ap()

    def sb(self, cols, dt=F32):
        n16 = cols * (2 if dt == F32 else 1)
        n16 += n16 % 2
        off = self.off
        self.off += n16
        assert self.off <= ARENA, f"SBUF arena overflow {self.off}"
        ap = self.arena[:, off:off + n16]
        if dt == F32:
            ap = ap.bitcast(F32)
        return ap

    def mark(self):
        return self.off

    def release(self, mark):
        self.P.barrier()
        self.off = mark

    def I(self, eng, meth, r, w, *a, pw=(), **kw):
        return self.P.op(eng, lambda e: getattr(e, meth)(*a, **kw), reads=r, writes=w, pwrites=pw)

    def dma(self, q, out, in_, r, w, grp, pw=()):
        return self.P.op(q, lambda e: e.dma_start(out=out, in_=in_), reads=r, writes=w, pwrites=pw, dma=grp)

    def finish(self):
        self.P.emit(self.st)
        self.st.close()
        return self.nc


def colblocks(T, bs=512):
    return [(c, min(c + bs, T)) for c in range(0, T, bs)]


def load_vec(kb, dram_ap, cols, q='sync', name='v'):
    t = kb.sb(cols, F32)
    tok = Tok(name)
    kb.nd = getattr(kb, 'nd', 0) + 1
    kb.dma(q, t, dram_ap, [], [tok], f"c{kb.nd % 4}")
    return t, tok


def linear(kb, W, nK, n_list, rhs_fn, blocks, epi, wpool, banks, kg=32, wcol0=0):
    nb = len(blocks)
    nset = len(banks) // nb
    it = getattr(kb, '_lin_it', 0)
    for n in n_list:
        bset = banks[(it % nset) * nb:(it % nset + 1) * nb]
        it += 1
        for k0 in range(0, nK, kg):
            k1 = min(k0 + kg, nK)
            wb, wt, wn = wpool.next()
            wv = wb[:, 0:(k1 - k0) * 128].rearrange("p (k n) -> p k n", n=128)
            src = W[k0 * 128:k1 * 128, wcol0 + n * 128:wcol0 + (n + 1) * 128].rearrange("(k p) n -> p k n", p=128)
            kb.dma('gpsimd', wv, src, [], [wt], "L" + wn)
            for k in range(k0, k1):
                for bi, (c0, c1) in enumerate(blocks):
                    rap, rtoks = rhs_fn(k, c0, c1)
                    b = bset[bi]
                    kb.I('tensor', 'matmul', [wt] + rtoks, [kb.pst[b]], kb.ps[b][:, 0:c1 - c0],
                         lhsT=wv[:, k - k0, :], rhs=rap, start=(k == 0), stop=(k == nK - 1))
        epi(n, [(kb.ps[bset[bi]][:, 0:c1 - c0], kb.pst[bset[bi]], c0, c1) for bi, (c0, c1) in enumerate(blocks)])
    kb._lin_it = it


def sumsq_finish(kb, acc, acc_tok, T, ones_f32, ones_tok, inv_n, out, out_tok, banks):
    for bi, (c0, c1) in enumerate(colblocks(T)):
        b = banks[bi % len(banks)]
        kb.I('tensor', 'matmul', [acc_tok, ones_tok], [kb.pst[b]], kb.ps[b][:, 0:c1 - c0],
             lhsT=ones_f32, rhs=acc[:, c0:c1], start=True, stop=True)
        kb.I('vector', 'tensor_scalar', [kb.pst[b]], [], out[:, c0:c1], kb.ps[b][:, 0:c1 - c0],
             inv_n, EPS, ALU.mult, ALU.add, pw=[out_tok])
    kb.I('scalar', 'activation', [out_tok], [out_tok], out[:, 0:T], out[:, 0:T], AF.Sqrt)
    kb.I('vector', 'reciprocal', [out_tok], [out_tok], out[:, 0:T], out[:, 0:T])


def modulate(kb, src_fn, T, A, B, vtoks, hT, h_tok, ones_f32, ones_tok, banks, xpool, hcol0=0, segs=None):
    if segs is None:
        segs = [(0, T, 0)]
    acc = kb.sb(T, F32)
    acc_t = Tok('acc')
    rstd = kb.sb(T, F32)
    rstd_t = Tok('rstd')
    tmp_pool = Pool(kb, 'mt', 2, T, F32)
    for k in range(KC):
        xb, xt, xn = xpool.next()
        kb.dma('sync', xb[:, 0:T], src_fn(k), [], [xt], "L" + xn)
        if k == 0:
            kb.I('vector', 'tensor_tensor', [xt], [acc_t], acc, xb[:, 0:T], xb[:, 0:T], ALU.mult)
        else:
            tb, tt, _ = tmp_pool.next()
            kb.I('scalar', 'activation', [xt], [tt], tb, xb[:, 0:T], AF.Square)
            kb.I('vector', 'tensor_tensor', [tt, acc_t], [acc_t], acc, acc, tb, ALU.add)
    sumsq_finish(kb, acc, acc_t, T, ones_f32, ones_tok, 1.0 / D, rstd, rstd_t, banks)
    for k in range(KC):
        xb, xt, xn = xpool.next()
        kb.dma('sync', xb[:, 0:T], src_fn(k), [], [xt], "L" + xn)
        tb, tt, _ = tmp_pool.next()
        for (c0, c1, vi) in segs:
            kb.I('vector', 'scalar_tensor_tensor', [xt, rstd_t] + vtoks, [], tb[:, c0:c1], xb[:, c0:c1], A[vi][:, k:k + 1],
                 rstd[:, c0:c1], ALU.mult, ALU.mult, pw=[tt])
        for (c0, c1, vi) in segs:
            kb.I('scalar', 'activation', [tt] + vtoks, [], hT[:, k, hcol0 + c0:hcol0 + c1], tb[:, c0:c1], AF.Identity,
                 bias=B[vi][:, k:k + 1], scale=1.0, pw=[h_tok])


def mod_vectors(kb, modv, gn, vtok):
    mv = modv.rearrange("p (v j k) -> p v j k", v=2, j=6)
    return mv


MODW = 6 * D // NCORES


def build_mod():
    kb = KB()
    cv = kb.din("cv", [128, KC * 2])
    w0 = kb.din("w0", [D, MODW])
    w1 = kb.din("w1", [D, MODW])
    b0 = kb.din("b0", [1, MODW])
    b1 = kb.din("b1", [1, MODW])
    out = kb.dout("modo", [2, 2, MODW])
    cs, ct = load_vec(kb, cv, KC * 2)
    sc = kb.sb(KC * 2, F32)
    sct = Tok('sc')
    kb.I('scalar', 'activation', [ct], [sct], sc, cs, AF.Silu)
    scv = sc.rearrange("p (k v) -> p k v", v=2)
    wpool = Pool(kb, 'mw', 2, KC * 512 * 2, BF16)
    res = kb.sb(MODW, F32)
    bia = kb.sb(MODW, F32)
    bank_i = 0
    for li, (w, b) in enumerate([(w0, b0), (w1, b1)]):
        bt = Tok('bias')
        rt = Tok('res')
        kb.dma('sync', bia[0:1, :], b, [], [], "bia", pw=[bt])
        kb.dma('sync', bia[1:2, :], b, [], [], "bia", pw=[bt])
        for cb in range(MODW // 512):
            wb, wt, wn = wpool.next()
            wv = wb.bitcast(F32).rearrange("p (k n) -> p k n", n=512)
            kb.dma('sync' if cb % 2 == 0 else 'scalar', wv,
                   w[:, cb * 512:(cb + 1) * 512].rearrange("(k p) n -> p k n", p=128), [], [wt], "L" + wn)
            bk = bank_i % 4
            bank_i += 1
            for k in range(KC):
                kb.I('tensor', 'matmul', [wt, sct], [kb.pst[bk]], kb.ps[bk][0:2, 0:512],
                     lhsT=scv[:, k, :], rhs=wv[:, k, :], start=(k == 0), stop=(k == KC - 1))
            kb.I('vector', 'tensor_tensor', [kb.pst[bk], bt], [], res[0:2, cb * 512:(cb + 1) * 512],
                 kb.ps[bk][0:2, 0:512], bia[0:2, cb * 512:(cb + 1) * 512], ALU.add, pw=[rt])
        kb.dma('sync', out[li], res[0:2, :], [rt], [], "out")
    return kb.finish()


_CACHE = {}


def get_prog(name, fn):
    if name not in _CACHE:
        _CACHE[name] = fn()
    return _CACHE[name]


def run(name, fn, in_maps):
    import time as _t
    t0 = _t.time()
    nc = get_prog(name, fn)
    t1 = _t.time()
    res = run_bass_kernel_spmd(nc, in_maps, core_ids=list(range(NCORES)))
    print(f"[launch {name}] build {t1 - t0:.1f}s run {_t.time() - t1:.1f}s", flush=True)
    return res.results


def fm(v):
    v = np.asarray(v, np.float32)
    lead = v.shape[:-1]
    return np.ascontiguousarray(np.moveaxis(v.reshape(lead + (KC, 128)), -1, 0))


def host_mod(c, c_ctx, w_mod0, b_mod0, w_mod1, b_mod1):
    cv = np.ascontiguousarray(np.stack([fm(c.reshape(-1)), fm(c_ctx.reshape(-1))], axis=-1).reshape(128, KC * 2))
    in_maps = []
    for i in range(NCORES):
        sl = slice(i * MODW, (i + 1) * MODW)
        in_maps.append({"cv": cv, "w0": np.ascontiguousarray(w_mod0[:, sl]), "w1": np.ascontiguousarray(w_mod1[:, sl]),
                        "b0": np.ascontiguousarray(b_mod0[sl].reshape(1, MODW)), "b1": np.ascontiguousarray(b_mod1[sl].reshape(1, MODW))})
    r = run("mod", build_mod, in_maps)
    full = np.concatenate([r[i]["modo"] for i in range(NCORES)], axis=-1)
    return full.reshape(2, 2, 6, D)


T0 = TL + TC
NQ = 32
NKCH = 20
VW = 2560


def consts_common(kb):
    c = {}
    c['ones_f'] = kb.din("ones_f", [128, 128])
    c['ones_b'] = kb.din("ones_b", [128, 128], BF16)
    of, oft = load_vec(kb, c['ones_f'], 128)
    ob = kb.sb(128, BF16)
    obt = Tok('ones_b')
    kb.dma('sync', ob, c['ones_b'], [], [obt], "cb")
    return of, oft, ob, obt


def host_consts():
    return {"ones_f": np.ones((128, 128), np.float32), "ones_b": np.ones((128, 128), NPBF)}


def build_l0a():
    kb = KB()
    xT = kb.din("xT", [D, T0])
    modv = kb.din("modv", [128, 2 * 6 * KC])
    gn = kb.din("gn", [128, KC])
    w_in = kb.din("w_in", [D, 9216])
    qkn = kb.din("qkn", [128, 6])
    cos = kb.din("cos", [128, TL])
    sin = kb.din("sin", [128, TL])
    rmat = kb.din("rmat", [128, 128], BF16)
    qT = kb.dout("qT", [NQ, 128, T0], BF16)
    kT = kb.dout("kT", [NKCH, 128, T0], BF16)
    Vo = kb.dout("V", [T0, VW], BF16)
    of, oft, ob, obt = consts_common(kb)
    mv_s, mvt = load_vec(kb, modv, 2 * 6 * KC)
    gn_s, gnt = load_vec(kb, gn, KC)
    qkn_s, qknt = load_vec(kb, qkn, 6)
    cos_s, cost = load_vec(kb, cos, TL)
    sin_s, sint = load_vec(kb, sin, TL)
    rm = kb.sb(128, BF16)
    rmt = Tok('rmat')
    kb.dma('sync', rm, rmat, [], [rmt], "cb")
    mv = mv_s.rearrange("p (v j k) -> p v j k", v=2, j=6)
    A = [kb.sb(KC, F32) for _ in range(2)]
    At = Tok('A')
    for v in range(2):
        kb.I('vector', 'scalar_tensor_tensor', [mvt, gnt], [], A[v], mv[:, v, 1, :], 1.0, gn_s, ALU.add, ALU.mult, pw=[At])
    Bv = [mv[:, v, 0, :] for v in range(2)]
    geff = kb.sb(6, F32)
    gefft = Tok('geff')
    kb.I('vector', 'tensor_copy', [qknt], [gefft], geff, qkn_s)
    for gi in (0, 2, 3):
        kb.I('vector', 'tensor_scalar', [gefft], [gefft], geff[:, gi:gi + 1], geff[:, gi:gi + 1], 128.0 ** -0.5, 0.0, ALU.mult, ALU.add)
    hT_flat = kb.sb(KC * T0, BF16)
    hT = hT_flat.rearrange("p (k t) -> p k t", k=KC)
    ht = Tok('hT')
    m0 = kb.mark()
    xpool = Pool(kb, 'xp', 2, T0, F32)
    modulate(kb, lambda k: xT[k * 128:(k + 1) * 128, :], T0, A, Bv, [At, mvt], hT, ht, of, oft, [6, 7], xpool,
             segs=[(0, TL, 0), (TL, T0, 1)])
    kb.release(m0)
    wpool = Pool(kb, 'wp', 4, KC * 128, BF16)
    upool = Pool(kb, 'u', 2, T0, F32)
    sq = kb.sb(T0, F32)
    sqt = Tok('sq')
    rstd = kb.sb(T0, F32)
    rstdt = Tok('rstd')
    unpool = Pool(kb, 'un', 2, T0, BF16)
    t1 = kb.sb(TL, F32)
    t1t = Tok('t1')
    t2 = kb.sb(TL, F32)
    t2t = Tok('t2')
    opool = Pool(kb, 'o', 2, TL, BF16)
    blocks = colblocks(T0)
    state = {'i': 0}

    def epi(n, tiles):
        if n < 16:
            gi, dst = 0, qT[n]
        elif n < 20:
            gi, dst = 1, kT[n - 16]
        elif n < 40:
            gi, dst = 2 + (n - 24) % 2, qT[16 + n - 24]
        else:
            gi, dst = 4 + (n - 40) % 2, kT[4 + n - 40]
        ub, ut, _ = upool.next()
        for (ps, pt, c0, c1) in tiles:
            kb.I('scalar', 'activation', [pt], [], ub[:, c0:c1], ps, AF.Copy, pw=[ut])
            kb.I('scalar', 'activation', [pt], [], sq[:, c0:c1], ps, AF.Square, pw=[sqt])
        for bi, (c0, c1) in enumerate(blocks):
            b = 6 + (state['i'] % 2)
            state['i'] += 1
            kb.I('tensor', 'matmul', [sqt, oft], [kb.pst[b]], kb.ps[b][:, 0:c1 - c0], lhsT=of, rhs=sq[:, c0:c1], start=True, stop=True)
            kb.I('vector', 'tensor_scalar', [kb.pst[b]], [], rstd[:, c0:c1], kb.ps[b][:, 0:c1 - c0], 1.0 / 128, EPS, ALU.mult, ALU.add,
                 pw=[rstdt])
        kb.I('scalar', 'activation', [rstdt], [rstdt], rstd, rstd, AF.Sqrt)
        kb.I('vector', 'reciprocal', [rstdt], [rstdt], rstd, rstd)
        unb, unt, unn = unpool.next()
        kb.I('vector', 'scalar_tensor_tensor', [ut, rstdt, gefft], [unt], unb, ub, geff[:, gi:gi + 1], rstd, ALU.mult, ALU.mult)
        obuf, otk, on = opool.next()
        kb.I('gpsimd', 'tensor_tensor', [unt, cost], [t1t], t1, unb[:, 0:TL], cos_s, ALU.mult)
        for bi, (c0, c1) in enumerate(colblocks(TL)):
            b = 6 + (state['i'] % 2)
            state['i'] += 1
            kb.I('tensor', 'matmul', [unt, rmt], [kb.pst[b]], kb.ps[b][:, 0:c1 - c0], lhsT=rm, rhs=unb[:, c0:c1], start=True, stop=True)
            kb.I('vector', 'tensor_tensor', [kb.pst[b], sint], [], t2[:, c0:c1], kb.ps[b][:, 0:c1 - c0], sin_s[:, c0:c1], ALU.mult, pw=[t2t])
        kb.I('vector', 'tensor_tensor', [t1t, t2t], [otk], obuf, t1, t2, ALU.add)
        kb.dma('sync', dst[:, 0:TL], obuf, [otk], [], "S" + on)
        kb.dma('sync', dst[:, TL:T0], unb[:, TL:T0], [unt], [], "S" + unn)

    n_list = list(range(0, 20)) + list(range(24, 56))
    linear(kb, w_in, KC, n_list, lambda k, c0, c1: (hT[:, k, c0:c1], [ht]), blocks, epi, wpool, [0, 1, 2, 3, 4, 5])
    kb.release(m0)
    vw = Pool(kb, 'vw', 2, KC * 512, BF16)
    vo = Pool(kb, 'vo', 3, 512, BF16)
    it = 0
    for cg in range(5):
        wcol = 2560 if cg == 0 else 7168 + (cg - 1) * 512
        wb, wt, wn = vw.next()
        wv = wb.rearrange("p (k n) -> p k n", n=512)
        kb.dma('gpsimd', wv, w_in[:, wcol:wcol + 512].rearrange("(k p) n -> p k n", p=128), [], [wt], "L" + wn)
        for tt in range(T0 // 128):
            b = it % 6
            it += 1
            for k in range(KC):
                kb.I('tensor', 'matmul', [wt, ht], [kb.pst[b]], kb.ps[b][:, 0:512], lhsT=hT[:, k, tt * 128:(tt + 1) * 128],
                     rhs=wv[:, k, :], start=(k == 0), stop=(k == KC - 1))
            vb, vt, vn = vo.next()
            if it % 2 == 0:
                kb.I('scalar', 'activation', [kb.pst[b]], [vt], vb, kb.ps[b][:, 0:512], AF.Copy)
            else:
                kb.I('vector', 'tensor_copy', [kb.pst[b]], [vt], vb, kb.ps[b][:, 0:512])
            kb.dma('sync', Vo[tt * 128:(tt + 1) * 128, cg * 512:(cg + 1) * 512], vb, [vt], [], "S" + vn)
    return kb.finish()


def rope_tables(core):
    half = 64
    inv = 10000.0 ** (-np.arange(0, half, 2, dtype=np.float32) / half)
    tok = np.arange(core * TL, (core + 1) * TL)
    row = (tok // 64).astype(np.float32)
    col = (tok % 64).astype(np.float32)
    ang = np.concatenate([row[:, None] * inv, col[:, None] * inv], axis=-1)
    ang = np.repeat(ang, 2, axis=1).T
    return np.ascontiguousarray(np.cos(ang).astype(np.float32)), np.ascontiguousarray(np.sin(ang).astype(np.float32))


def rot_matrix():
    r = np.zeros((128, 128), np.float32)
    for i in range(64):
        r[2 * i + 1, 2 * i] = -1.0
        r[2 * i, 2 * i + 1] = 1.0
    return r.astype(NPBF)


def modv_layout(mod_l):
    return np.ascontiguousarray(fm(mod_l).reshape(128, 2 * 6 * KC))


def host_l0a(x, ctx, mod0, norm_mix, w_in, qn_a, kn_a, qn_b, kn_b):
    xTf = np.ascontiguousarray(x.T)
    cT = np.ascontiguousarray(ctx.T)
    qkn = np.ascontiguousarray(np.stack([qn_a, kn_a, qn_b[0], qn_b[1], kn_b[0], kn_b[1]], axis=1).astype(np.float32))
    common = dict(host_consts(), modv=modv_layout(mod0), gn=fm(norm_mix), w_in=w_in, qkn=qkn, rmat=rot_matrix())
    in_maps = []
    for i in range(NCORES):
        cs, sn = rope_tables(i)
        m = dict(common)
        m["xT"] = np.ascontiguousarray(np.concatenate([xTf[:, i * TL:(i + 1) * TL], cT], axis=1))
        m["cos"] = cs
        m["sin"] = sn
        in_maps.append(m)
    return run("l0a", build_l0a, in_maps)


NKEY = TC + S
NKT = NKEY // 128
DFF = 11008
NF = DFF // 128
LAM_INIT0 = 0.8 - 0.6 * float(np.exp(-0.3 * 0))


def attn_unit(kb, q_ap, qtok, kTs, ktok, Vs, vtok, ndv, outs, outtok, ob, obt, ptpool, rz, rzt):
    SB = [0, 1, 2]
    OB = [3, 4]
    ZB = 5
    for (c0, c1, nkt) in [(0, 512, NKT), (512, 1024, NKT), (1024, 1280, 2)]:
        w = c1 - c0

        def s_mm(kt):
            b = SB[kt % 3]
            kb.I('tensor', 'matmul', [ktok, qtok], [kb.pst[b]], kb.ps[b][:, 0:w], lhsT=kTs[:, kt * 128:(kt + 1) * 128],
                 rhs=q_ap[:, c0:c1], start=True, stop=True)
        s_mm(0)
        for kt in range(nkt):
            if kt + 1 < nkt:
                s_mm(kt + 1)
            b = SB[kt % 3]
            pb, pt, _ = ptpool.next()
            kb.I('scalar', 'activation', [kb.pst[b]], [pt], pb[:, 0:w], kb.ps[b][:, 0:w], AF.Exp)
            for j in range(ndv):
                kb.I('tensor', 'matmul', [vtok, pt], [kb.pst[OB[j]]], kb.ps[OB[j]][:, 0:w], lhsT=Vs[:, kt, j * 128:(j + 1) * 128],
                     rhs=pb[:, 0:w], start=(kt == 0), stop=(kt == nkt - 1))
            kb.I('tensor', 'matmul', [obt, pt], [kb.pst[ZB]], kb.ps[ZB][:, 0:w], lhsT=ob, rhs=pb[:, 0:w],
                 start=(kt == 0), stop=(kt == nkt - 1))
        kb.I('vector', 'reciprocal', [kb.pst[ZB]], [rzt], rz[:, 0:w], kb.ps[ZB][:, 0:w])
        for j in range(ndv):
            kb.I('vector', 'tensor_tensor', [kb.pst[OB[j]], rzt], [], outs[j][:, c0:c1], kb.ps[OB[j]][:, 0:w], rz[:, 0:w], ALU.mult,
                 pw=[outtok[j]])


def build_l0b1():
    kb = KB()
    qT = kb.din("qT", [NQ, 128, T0], BF16)
    kT = kb.din("kT", [NKCH, 128, NKEY], BF16)
    Vd = kb.din("V", [NKEY, VW], BF16)
    lamv = kb.din("lamv", [128, 4])
    subln = kb.din("subln", [128, 2])
    cat = kb.dout("cat", [KC, 128, T0], BF16)
    cat_t = Tok('cat')
    x0a_t = Tok('x0a')
    of, oft, ob, obt = consts_common(kb)
    lam_s, lamt = load_vec(kb, lamv, 4)
    sub_s, subt = load_vec(kb, subln, 2)
    lp = kb.sb(2, F32)
    lpt = Tok('lp')
    kb.I('vector', 'tensor_tensor', [lamt], [], lp[:, 0:1], lam_s[:, 0:1], lam_s[:, 1:2], ALU.mult, pw=[lpt])
    kb.I('vector', 'tensor_tensor', [lamt], [], lp[:, 1:2], lam_s[:, 2:3], lam_s[:, 3:4], ALU.mult, pw=[lpt])
    kb.I('tensor', 'matmul', [lpt, oft], [kb.pst[7]], kb.ps[7][:, 0:2], lhsT=of, rhs=lp, start=True, stop=True)
    le = kb.sb(2, F32)
    let = Tok('le')
    kb.I('scalar', 'activation', [kb.pst[7]], [let], le, kb.ps[7][:, 0:2], AF.Exp)
    nlam = kb.sb(1, F32)
    nlt = Tok('nlam')
    kb.I('vector', 'tensor_tensor', [let], [nlt], nlam, le[:, 1:2], le[:, 0:1], ALU.subtract)
    kb.I('vector', 'tensor_scalar', [nlt], [nlt], nlam, nlam, -LAM_INIT0, 0.0, ALU.add, ALU.add)
    subg = kb.sb(2, F32)
    subgt = Tok('subg')
    kb.I('vector', 'tensor_scalar', [subt], [subgt], subg, sub_s, 1.0 - LAM_INIT0, 0.0, ALU.mult, ALU.add)
    m0 = kb.mark()
    kpool = Pool(kb, 'kp', 2, NKEY, BF16)
    vpool = Pool(kb, 'vp', 2, NKT * 256, BF16)
    qpool = Pool(kb, 'qp', 2, T0, BF16)
    ptpool = Pool(kb, 'pt', 3, 512, BF16)
    rz = kb.sb(512, F32)
    rzt = Tok('rz')
    o1 = [kb.sb(T0, F32) for _ in range(2)]
    o1t = [Tok('o1a'), Tok('o1b')]
    o2 = [kb.sb(T0, F32) for _ in range(2)]
    o2t = [Tok('o2a'), Tok('o2b')]
    sq = kb.sb(T0, F32)
    sqt = Tok('sq')
    rstd = kb.sb(T0, F32)
    rstdt = Tok('rstd')
    cpool = Pool(kb, 'cs', 2, T0, BF16)
    for g in range(4):
        kb_, kt_, kn_ = kpool.next()
        kb.dma('sync', kb_, kT[g], [], [kt_], "L" + kn_)
        vb_, vt_, vn_ = vpool.next()
        Vs = vb_[:, 0:NKT * 128].rearrange("p (t c) -> p t c", c=128)
        for hf in range(2):
            kb.dma('gpsimd', Vs[:, hf * 33:(hf + 1) * 33, :],
                   Vd[hf * 33 * 128:(hf + 1) * 33 * 128, g * 128:(g + 1) * 128].rearrange("(t p) c -> p t c", p=128), [], [], "L" + vn_, pw=[vt_])
        for hh in range(4):
            h = g * 4 + hh
            qb_, qt_, qn_ = qpool.next()
            kb.dma('sync', qb_, qT[h], [], [qt_], "L" + qn_)
            attn_unit(kb, qb_, qt_, kb_, kt_, Vs, vt_, 1, [o1[0]], [o1t[0]], ob, obt, ptpool, rz, rzt)
            cb_, ct_, cn_ = cpool.next()
            kb.I('scalar', 'activation', [o1t[0]], [ct_], cb_, o1[0], AF.Copy)
            kb.dma('sync', cat[h], cb_, [ct_], [], "S" + cn_, pw=[cat_t])
    for h in range(8):
        vb_, vt_, vn_ = vpool.next()
        Vs = vb_.rearrange("p (t c) -> p t c", c=256)
        for hf in range(2):
            kb.dma('gpsimd', Vs[:, hf * 33:(hf + 1) * 33, :],
                   Vd[hf * 33 * 128:(hf + 1) * 33 * 128, 512 + h * 256:512 + (h + 1) * 256].rearrange("(t p) c -> p t c", p=128), [], [], "L" + vn_, pw=[vt_])
        for m in range(2):
            kb_, kt_, kn_ = kpool.next()
            kb.dma('sync', kb_, kT[4 + h * 2 + m], [], [kt_], "L" + kn_)
            qb_, qt_, qn_ = qpool.next()
            kb.dma('sync', qb_, qT[16 + h * 2 + m], [], [qt_], "L" + qn_)
            oo, oot = (o1, o1t) if m == 0 else (o2, o2t)
            attn_unit(kb, qb_, qt_, kb_, kt_, Vs, vt_, 2, oo, oot, ob, obt, ptpool, rz, rzt)
        for j in range(2):
            kb.I('vector', 'scalar_tensor_tensor', [o1t[j], o2t[j], nlt], [o1t[j]], o1[j], o2[j], nlam[:, 0:1], o1[j], ALU.mult, ALU.add)
        for bi, (c0, c1) in enumerate(colblocks(T0)):
            b = 6 + bi % 2
            for j in range(2):
                kb.I('scalar', 'activation', [o1t[j]], [sqt], sq[:, c0:c1], o1[j][:, c0:c1], AF.Square)
                kb.I('tensor', 'matmul', [sqt, oft], [kb.pst[b]], kb.ps[b][:, 0:c1 - c0], lhsT=of, rhs=sq[:, c0:c1],
                     start=(j == 0), stop=(j == 1))
            kb.I('vector', 'tensor_scalar', [kb.pst[b]], [], rstd[:, c0:c1], kb.ps[b][:, 0:c1 - c0], 1.0 / 256, EPS, ALU.mult, ALU.add,
                 pw=[rstdt])
        kb.I('scalar', 'activation', [rstdt], [rstdt], rstd, rstd, AF.Sqrt)
        kb.I('vector', 'reciprocal', [rstdt], [rstdt], rstd, rstd)
        for j in range(2):
            cb_, ct_, cn_ = cpool.next()
            kb.I('vector', 'scalar_tensor_tensor', [o1t[j], rstdt, subgt], [ct_], cb_, o1[j], subg[:, j:j + 1], rstd, ALU.mult, ALU.mult)
            kb.dma('sync', cat[16 + h * 2 + j], cb_, [ct_], [], "S" + cn_, pw=[cat_t])
    return kb.finish()


def build_l0b2():
    kb = KB()
    cat = kb.din("cat", [KC, 128, T0], BF16)
    w_out = kb.din("w_out", [D, D])
    xT = kb.din("xT", [D, T0])
    modv = kb.din("modv", [128, 2 * 6 * KC])
    gn2 = kb.din("gn2", [128, KC])
    wg = kb.din("wg", [D, DFF])
    wu = kb.din("wu", [D, DFF])
    wd = kb.din("wd", [DFF, D])
    x1T = kb.dout("x1T", [D, T0])
    x0a = kb.dscr("x0a", [KC, 128, T0], F32)
    cat_t = Tok('cat')
    x0a_t = Tok('x0a')
    of, oft, ob, obt = consts_common(kb)
    mv_s, mvt = load_vec(kb, modv, 2 * 6 * KC)
    gn2_s, gn2t = load_vec(kb, gn2, KC)
    mv = mv_s.rearrange("p (v j k) -> p v j k", v=2, j=6)
    m0 = kb.mark()
    catT_flat = kb.sb(KC * T0, BF16)
    catT = catT_flat.rearrange("p (k t) -> p k t", k=KC)
    catT_t = Tok('catT')
    for k in range(KC):
        kb.dma('sync', catT[:, k, :], cat[k], [cat_t], [], f"Lcat{k % 2}", pw=[catT_t])
    wpool = Pool(kb, 'wp', 4, KC * 128, BF16)
    xpool = Pool(kb, 'xp', 2, T0, F32)
    spool = Pool(kb, 'st', 2, T0, F32)
    acc = kb.sb(T0, F32)
    acct = Tok('acc')
    tmp = kb.sb(T0, F32)
    tmpt = Tok('tmp')
    rstd2 = kb.sb(T0, F32)
    rstd2t = Tok('rstd2')
    blocks = colblocks(T0)
    seg_of = [0, 0, 1]

    def epi_out(n, tiles):
        xb, xt, xn = xpool.next()
        kb.dma('sync', xb, xT[n * 128:(n + 1) * 128, :], [], [xt], "L" + xn)
        sb_, st_, sn_ = spool.next()
        for bi, (ps, pt, c0, c1) in enumerate(tiles):
            v = seg_of[bi]
            kb.I('vector', 'scalar_tensor_tensor', [pt, xt, mvt], [], sb_[:, c0:c1], ps, mv[:, v, 2, n:n + 1], xb[:, c0:c1],
                 ALU.mult, ALU.add, pw=[st_])
        kb.dma('sync', x0a[n], sb_, [st_], [], "S" + sn_, pw=[x0a_t])
        if n == 0:
            kb.I('gpsimd', 'tensor_tensor', [st_], [acct], acc, sb_, sb_, ALU.mult)
        else:
            kb.I('scalar', 'activation', [st_], [tmpt], tmp, sb_, AF.Square)
            kb.I('gpsimd', 'tensor_tensor', [tmpt, acct], [acct], acc, acc, tmp, ALU.add)

    linear(kb, w_out, KC, list(range(KC)), lambda k, c0, c1: (catT[:, k, c0:c1], [catT_t]), blocks, epi_out, wpool, [0, 1, 2, 3, 4, 5])
    kb.P.barrier()
    rstd2p = rstd2
    sumsq_finish(kb, acc, acct, T0, of, oft, 1.0 / D, rstd2p, rstd2t, [6, 7])
    A2 = [kb.sb(KC, F32) for _ in range(2)]
    A2t = Tok('A2')
    for v in range(2):
        kb.I('vector', 'scalar_tensor_tensor', [mvt, gn2t], [], A2[v], mv[:, v, 4, :], 1.0, gn2_s, ALU.add, ALU.mult, pw=[A2t])
    kb.P.barrier()
    kb.off = m0
    rstd2k = kb.sb(T0, F32)
    A2k = [kb.sb(KC, F32) for _ in range(2)]
    keept = Tok('keep')
    kb.I('vector', 'tensor_copy', [rstd2t], [], rstd2k, rstd2p, pw=[keept])
    for v in range(2):
        kb.I('vector', 'tensor_copy', [A2t], [], A2k[v], A2[v], pw=[keept])
    kb.P.barrier()
    m1 = kb.mark()
    for ti, (c0, c1) in enumerate([(0, 512), (512, 1024), (1024, 1280)]):
        v = seg_of[ti]
        Tt = c1 - c0
        h2_flat = kb.sb(KC * Tt, BF16)
        h2 = h2_flat.rearrange("p (k t) -> p k t", k=KC)
        h2t = Tok('h2')
        aT_flat = kb.sb(NF * Tt, BF16)
        aT = aT_flat.rearrange("p (f t) -> p f t", f=NF)
        aTt = Tok('aT')
        wpool = Pool(kb, 'wp', 6, KC * 128, BF16)
        xpool = Pool(kb, 'xp', 3, Tt, F32)
        tpool = Pool(kb, 'tp', 2, Tt, F32)
        spool = Pool(kb, 'st', 2, Tt, F32)
        for k in range(KC):
            xb, xt, xn = xpool.next()
            kb.dma('sync', xb, x0a[k][:, c0:c1], [x0a_t], [xt], "L" + xn)
            tb, tt_, _ = tpool.next()
            kb.I('vector', 'scalar_tensor_tensor', [xt, keept], [tt_], tb, xb, A2k[v][:, k:k + 1], rstd2k[:, c0:c1], ALU.mult, ALU.mult)
            kb.I('scalar', 'activation', [tt_, mvt], [], h2[:, k, :], tb, AF.Identity, bias=mv[:, v, 3, k:k + 1], scale=1.0, pw=[h2t])
        for f in range(NF):
            pair = (f % 3) * 2
            for (W, b) in ((wg, pair), (wu, pair + 1)):
                wb, wt, wn = wpool.next()
                wv = wb.rearrange("p (k n) -> p k n", n=128)
                kb.dma('gpsimd', wv, W[:, f * 128:(f + 1) * 128].rearrange("(k p) n -> p k n", p=128), [], [wt], "L" + wn)
                for k in range(KC):
                    kb.I('tensor', 'matmul', [wt, h2t], [kb.pst[b]], kb.ps[b][:, 0:Tt], lhsT=wv[:, k, :], rhs=h2[:, k, :],
                         start=(k == 0), stop=(k == KC - 1))
            tb, tt_, _ = tpool.next()
            kb.I('scalar', 'activation', [kb.pst[pair]], [tt_], tb, kb.ps[pair][:, 0:Tt], AF.Silu)
            kb.I('vector', 'tensor_tensor', [tt_, kb.pst[pair + 1]], [], aT[:, f, :], tb, kb.ps[pair + 1][:, 0:Tt], ALU.mult, pw=[aTt])

        def epi_dn(n, tiles, c0=c0, c1=c1, v=v, xpool=xpool, spool=spool):
            ps, pt, _, _ = tiles[0]
            xb, xt, xn = xpool.next()
            kb.dma('sync', xb, x0a[n][:, c0:c1], [x0a_t], [xt], "L" + xn)
            sb_, st_, sn_ = spool.next()
            kb.I('vector', 'scalar_tensor_tensor', [pt, xt, mvt], [st_], sb_, ps, mv[:, v, 5, n:n + 1], xb, ALU.mult, ALU.add)
            kb.dma('sync', x1T[n * 128:(n + 1) * 128, c0:c1], sb_, [st_], [], "S" + sn_)

        linear(kb, wd, NF, list(range(KC)), lambda k, a, b_, aT=aT, aTt=aTt: (aT[:, k, a:b_], [aTt]), [(0, Tt)], epi_dn, wpool,
               [0, 1, 2, 3, 4, 5])
        kb.release(m1)
    return kb.finish()


def host_l0b1(r0a, lq1, lk1, lq2, lk2, subln):
    kT_all = np.ascontiguousarray(np.concatenate([r0a[0]["kT"][:, :, TL:T0]] + [r0a[i]["kT"][:, :, 0:TL] for i in range(NCORES)], axis=2))
    V_all = np.ascontiguousarray(np.concatenate([r0a[0]["V"][TL:T0]] + [r0a[i]["V"][0:TL] for i in range(NCORES)], axis=0))
    lamv = np.ascontiguousarray(np.stack([lq1, lk1, lq2, lk2], axis=1).astype(np.float32))
    sub = np.ascontiguousarray(subln.reshape(2, 128).T.astype(np.float32))
    common = dict(host_consts(), kT=kT_all, V=V_all, lamv=lamv, subln=sub)
    in_maps = []
    for i in range(NCORES):
        m = dict(common)
        m["qT"] = np.ascontiguousarray(r0a[i]["qT"])
        in_maps.append(m)
    return run("l0b1", build_l0b1, in_maps)


def host_l0b2(r0b1, x, ctx, mod0, norm_ffn, w_out, wg, wu, wd):
    xTf = np.ascontiguousarray(x.T)
    cT = np.ascontiguousarray(ctx.T)
    common = dict(host_consts(), w_out=w_out, modv=modv_layout(mod0), gn2=fm(norm_ffn), wg=wg, wu=wu, wd=wd)
    in_maps = []
    for i in range(NCORES):
        m = dict(common)
        m["cat"] = np.ascontiguousarray(r0b1[i]["cat"])
        m["xT"] = np.ascontiguousarray(np.concatenate([xTf[:, i * TL:(i + 1) * TL], cT], axis=1))
        in_maps.append(m)
    return run("l0b2", build_l0b2, in_maps)


TE = TL + 3
T1 = TE + TC
XRW = TE + TC + 3


def build_l1a():
    kb = KB()
    xT = kb.din("xT", [D, T1])
    modv = kb.din("modv", [128, 2 * 6 * KC])
    gn = kb.din("gn", [128, KC])
    w_in = kb.din("w_in", [D, 2 * D])
    hmask = kb.din("hmask", [128, TE])
    convw = kb.din("convw", [128, 4 * KC])
    convb = kb.din("convb", [128, KC])
    gaw = kb.din("gaw", [2, 16, 256, 256])
    gxw = kb.din("gxw", [2, 16, 256, 256])
    gab = kb.din("gab", [128, 2 * KC])
    gxb = kb.din("gxb", [128, 2 * KC])
    lam = kb.din("lam", [128, 2 * KC])
    G = kb.dout("G", [KC, 128, TL], BF16)
    S0 = kb.dout("S0", [KC, 128, TL])
    Pf = kb.dout("Pf", [KC, 128, TL])
    Pb = kb.dout("Pb", [KC, 128, TL])
    AE = kb.dout("AE", [128, KC * 6])
    xc = kb.dscr("xc", [KC, 128, TL + TC])
    xc_t = Tok('xc')
    of, oft, ob, obt = consts_common(kb)
    mv_s, mvt = load_vec(kb, modv, 2 * 6 * KC)
    gn_s, gnt = load_vec(kb, gn, KC)
    hm_s, hmt = load_vec(kb, hmask, TE)
    cw_s, cwt = load_vec(kb, convw, 4 * KC)
    cb_s, cbt = load_vec(kb, convb, KC)
    gab_s, gabt = load_vec(kb, gab, 2 * KC)
    gxb_s, gxbt = load_vec(kb, gxb, 2 * KC)
    lam_s, lamt = load_vec(kb, lam, 2 * KC)
    cw = cw_s.rearrange("p (j k) -> p j k", j=4)
    mv = mv_s.rearrange("p (v j k) -> p v j k", v=2, j=6)
    A = [kb.sb(KC, F32) for _ in range(2)]
    At = Tok('A')
    for v in range(2):
        kb.I('vector', 'scalar_tensor_tensor', [mvt, gnt], [], A[v], mv[:, v, 1, :], 1.0, gn_s, ALU.add, ALU.mult, pw=[At])
    Bv = [mv[:, v, 0, :] for v in range(2)]
    cvec = kb.sb(2 * KC, F32)
    cvt = Tok('cvec')
    kb.I('scalar', 'activation', [lamt], [cvt], cvec, lam_s, AF.Exp, scale=-1.0)
    kb.I('scalar', 'activation', [cvt], [cvt], cvec, cvec, AF.Ln, bias=1.0, scale=1.0)
    kb.I('vector', 'tensor_scalar', [cvt], [cvt], cvec, cvec, -8.0, 0.0, ALU.mult, ALU.add)
    aeb = kb.sb(KC * 6, F32)
    aet = Tok('ae')
    aev = aeb.rearrange("p (k j) -> p k j", j=6)
    zeros = kb.sb(TL, F32)
    zt = Tok('zeros')
    kb.I('vector', 'memset', [], [zt], zeros, 0.0)
    hT_flat = kb.sb(KC * T1, BF16)
    hT = hT_flat.rearrange("p (k t) -> p k t", k=KC)
    ht = Tok('hT')
    m0 = kb.mark()
    xpool = Pool(kb, 'xp', 2, T1, F32)
    modulate(kb, lambda k: xT[k * 128:(k + 1) * 128, :], T1, A, Bv, [At, mvt], hT, ht, of, oft, [6, 7], xpool,
             segs=[(0, TE, 0), (TE, T1, 1)])
    kb.release(m0)
    wpool = Pool(kb, 'wp', 4, KC * 128, BF16)
    gpool = Pool(kb, 'gs', 2, TL, BF16)

    def epi_y(n, tiles):
        gb, gt, gn_ = gpool.next()
        for (ps, pt, c0, c1) in tiles:
            kb.I('scalar', 'activation', [pt], [], gb[:, c0:c1], ps, AF.Gelu_apprx_tanh, pw=[gt])
        kb.dma('sync', G[n], gb, [gt], [], "S" + gn_)

    linear(kb, w_in, KC, list(range(KC)), lambda k, c0, c1: (hT[:, k, 2 + c0:2 + c1], [ht]), colblocks(TL), epi_y, wpool,
           [0, 1, 2, 3, 4, 5])
    xrpool = Pool(kb, 'xr', 2, XRW, F32)
    for i in range(2):
        kb.I('vector', 'memset', [], [], xrpool.bufs[i][:, TE:TE + 2], 0.0, pw=[xrpool.toks[i]])
        kb.I('vector', 'memset', [], [], xrpool.bufs[i][:, XRW - 1:XRW], 0.0, pw=[xrpool.toks[i]])
    xcpool = Pool(kb, 'xc', 2, TL + TC, F32)

    def epi_xr(n, tiles):
        k = n - KC
        xb, xt, _ = xrpool.next()
        for bi, (ps, pt, c0, c1) in enumerate(tiles):
            if bi < 2:
                kb.I('vector', 'tensor_tensor', [pt, hmt], [], xb[:, c0:c1], ps, hm_s[:, c0:c1], ALU.mult, pw=[xt])
            else:
                kb.I('vector', 'tensor_tensor', [pt, hmt], [], xb[:, 1024:TE], ps[:, 0:3], hm_s[:, 1024:TE], ALU.mult, pw=[xt])
                kb.I('scalar', 'activation', [pt], [], xb[:, TE + 2:TE + 2 + TC], ps[:, 3:3 + TC], AF.Copy, pw=[xt])
        cbuf, ct, cn = xcpool.next()
        for (o0, o1, i0) in [(0, TL, 0), (TL, TL + TC, TE)]:
            w = o1 - o0
            kb.I('scalar', 'activation', [xt, cwt, cbt], [], cbuf[:, o0:o1], xb[:, i0:i0 + w], AF.Identity,
                 bias=cb_s[:, k:k + 1], scale=cw[:, 0, k:k + 1], pw=[ct])
            for j in range(1, 4):
                kb.I('vector', 'scalar_tensor_tensor', [xt, cwt, ct], [ct], cbuf[:, o0:o1], xb[:, i0 + j:i0 + j + w], cw[:, j, k:k + 1],
                     cbuf[:, o0:o1], ALU.mult, ALU.add)
        kb.dma('sync', xc[k], cbuf, [ct], [], "S" + cn, pw=[xc_t])

    linear(kb, w_in, KC, list(range(KC, 2 * KC)), lambda k, c0, c1: (hT[:, k, c0:c1], [ht]), colblocks(T1), epi_xr, wpool,
           [0, 1, 2, 3, 4, 5])
    kb.off = m0 - KC * T1
    kb.P.barrier()
    TT = TL + TC
    xcl = Pool(kb, 'xl', 4, TT, F32)
    xcb = Pool(kb, 'xb', 4, TT, BF16)
    gwp = Pool(kb, 'gw', 4, 2 * 256, BF16)
    rbuf = kb.sb(TT, F32)
    rt = Tok('r')
    ibuf = kb.sb(TT, F32)
    it_ = Tok('i')
    abuf = kb.sb(TT, F32)
    at_ = Tok('a')
    a2buf = kb.sb(TT, F32)
    a2t = Tok('a2')
    bbuf = kb.sb(TT, F32)
    bt_ = Tok('b')
    hcb = kb.sb(TC, F32)
    hct = Tok('hc')
    hlb = kb.sb(TL, F32)
    hlt = Tok('hl')
    s0p = Pool(kb, 's0', 2, TL, F32)
    pfp = Pool(kb, 'pf', 2, TL, F32)
    pbp = Pool(kb, 'pb', 2, TL, F32)
    blocks = colblocks(TT)
    for blk in range(16):
        xl = []
        xbf = []
        for kk in range(2):
            lb, lt, ln_ = xcl.next()
            kb.dma('sync', lb, xc[2 * blk + kk], [xc_t], [lt], "L" + ln_)
            bb_, btk, _ = xcb.next()
            kb.I('scalar', 'activation', [lt], [btk], bb_, lb, AF.Copy)
            xl.append((lb, lt))
            xbf.append((bb_, btk))
        gws = {}
        for d in range(2):
            for gi, Wsrc in enumerate((gaw, gxw)):
                wb, wt, wn = gwp.next()
                wv = wb.rearrange("p (k n) -> p k n", n=256)
                kb.dma('gpsimd', wv, Wsrc[d, blk].rearrange("(k p) n -> p k n", p=128), [], [wt], "L" + wn)
                gws[(d, gi)] = (wv, wt)
        for e in range(2):
            c = 2 * blk + e
            s0b, s0t, s0n = s0p.next()
            pfb, pft, pfn = pfp.next()
            pbb, pbt, pbn = pbp.next()
            for d in range(2):
                for gi, (dst, dtok, bias_s, btok) in enumerate(((rbuf, rt, gab_s, gabt), (ibuf, it_, gxb_s, gxbt))):
                    wv, wt = gws[(d, gi)]
                    for bi, (c0, c1) in enumerate(blocks):
                        b = gi * 3 + bi
                        for kk in range(2):
                            kb.I('tensor', 'matmul', [wt, xbf[kk][1]], [kb.pst[b]], kb.ps[b][:, 0:c1 - c0],
                                 lhsT=wv[:, kk, e * 128:(e + 1) * 128], rhs=xbf[kk][0][:, c0:c1], start=(kk == 0), stop=(kk == 1))
                        kb.I('scalar', 'activation', [kb.pst[b], btok], [], dst[:, c0:c1], kb.ps[b][:, 0:c1 - c0], AF.Sigmoid,
                             bias=bias_s[:, d * KC + c:d * KC + c + 1], scale=1.0, pw=[dtok])
                kb.I('scalar', 'activation', [rt, cvt], [at_], abuf, rbuf, AF.Exp, scale=cvec[:, d * KC + c:d * KC + c + 1])
                kb.I('vector', 'tensor_tensor', [at_], [a2t], a2buf, abuf, abuf, ALU.mult)
                kb.I('scalar', 'activation', [a2t], [a2t], a2buf, a2buf, AF.Sqrt, bias=1.0, scale=-1.0)
                kb.I('vector', 'tensor_tensor', [it_, xl[e][1]], [bt_], bbuf, ibuf, xl[e][0], ALU.mult)
                kb.I('vector', 'tensor_tensor', [bt_, a2t], [bt_], bbuf, bbuf, a2buf, ALU.mult)
                rv = (lambda ap: ap) if d == 0 else (lambda ap: ap[:, ::-1])
                kb.I('vector', 'tensor_tensor_scan', [at_, bt_], [hct], rv(hcb), rv(abuf[:, TL:TT]), rv(bbuf[:, TL:TT]), 0.0, ALU.mult, ALU.add)
                hdst, hdt = (s0b, s0t) if d == 0 else (hlb, hlt)
                pdst, pdt = (pfb, pft) if d == 0 else (pbb, pbt)
                kb.I('vector', 'tensor_tensor_scan', [at_, bt_], [hdt], rv(hdst), rv(abuf[:, 0:TL]), rv(bbuf[:, 0:TL]), 0.0, ALU.mult, ALU.add)
                kb.I('vector', 'tensor_tensor_scan', [at_, zt], [pdt], rv(pdst), rv(abuf[:, 0:TL]), rv(zeros), 1.0, ALU.mult, ALU.add)
                endc = TL - 1 if d == 0 else 0
                kb.I('vector', 'tensor_copy', [pdt], [], aev[:, c, 2 * d:2 * d + 1], pdst[:, endc:endc + 1], pw=[aet])
                kb.I('vector', 'tensor_copy', [hdt], [], aev[:, c, 2 * d + 1:2 * d + 2], hdst[:, endc:endc + 1], pw=[aet])
                ce = TC - 1 if d == 0 else 0
                kb.I('vector', 'tensor_copy', [hct], [], aev[:, c, 4 + d:5 + d], hcb[:, ce:ce + 1], pw=[aet])
                if d == 1:
                    kb.I('vector', 'tensor_tensor', [s0t, hlt], [s0t], s0b, s0b, hlb, ALU.add)
            kb.dma('sync', S0[c], s0b, [s0t], [], "S" + s0n)
            kb.dma('sync', Pf[c], pfb, [pft], [], "S" + pfn)
            kb.dma('sync', Pb[c], pbb, [pbt], [], "S" + pbn)
    kb.dma('sync', AE, aeb, [aet], [], "Sae")
    return kb.finish()


def host_l1a(x1_lat, ctx1, mod1, norm_mix, w_in, conv_w, conv_b, ga_w, ga_b, gx_w, gx_b, lru_lam):
    xTf = np.ascontiguousarray(x1_lat.T)
    cT = np.ascontiguousarray(ctx1.T)
    xpad = np.concatenate([np.zeros((D, 2), np.float32), xTf, np.zeros((D, 1), np.float32)], axis=1)
    common = dict(host_consts(), modv=modv_layout(mod1), gn=fm(norm_mix), w_in=w_in,
                  convw=np.ascontiguousarray(fm(conv_w).reshape(128, 4 * KC)), convb=fm(conv_b),
                  gaw=ga_w, gxw=gx_w, gab=np.ascontiguousarray(fm(ga_b).reshape(128, 2 * KC)),
                  gxb=np.ascontiguousarray(fm(gx_b).reshape(128, 2 * KC)), lam=np.ascontiguousarray(fm(lru_lam).reshape(128, 2 * KC)))
    in_maps = []
    for i in range(NCORES):
        m = dict(common)
        m["xT"] = np.ascontiguousarray(np.concatenate([xpad[:, i * TL:i * TL + TE], cT], axis=1))
        hm = np.ones((128, TE), np.float32)
        if i == 0:
            hm[:, 0:2] = 0.0
        if i == NCORES - 1:
            hm[:, TE - 1:TE] = 0.0
        m["hmask"] = hm
        in_maps.append(m)
    return run("l1a", build_l1a, in_maps)


NE = 8
FH = 16


def build_l1b():
    kb = KB()
    S0 = kb.din("S0", [KC, 128, TL])
    Pf = kb.din("Pf", [KC, 128, TL])
    Pb = kb.din("Pb", [KC, 128, TL])
    G = kb.din("G", [KC, 128, TL], BF16)
    AEa = kb.din("AEa", [128, NCORES * KC * 6])
    sel = kb.din("sel", [128, NCORES])
    x1T = kb.din("x1T", [D, TL])
    modv = kb.din("modv", [128, 2 * 6 * KC])
    gn2 = kb.din("gn2", [128, KC])
    w_out = kb.din("w_out", [D, D])
    rw = kb.din("rw", [128, KC * NE])
    rb = kb.din("rb", [NE, 1])
    ident = kb.din("ident", [128, 128])
    outT = kb.dout("outT", [D, TL])
    h3o = kb.dout("h3T", [KC, 128, TL], BF16)
    comb_o = kb.dout("comb", [128, (TL // 128) * NE])
    mask_o = kb.dout("mask", [128, (TL // 128) * NE])
    out_t = [Tok(f'out{n}') for n in range(KC)]
    of, oft, ob, obt = consts_common(kb)
    mv_s, mvt = load_vec(kb, modv, 2 * 6 * KC)
    gn2_s, gn2t = load_vec(kb, gn2, KC)
    ae_s, aet = load_vec(kb, AEa, NCORES * KC * 6)
    sel_s, selt = load_vec(kb, sel, NCORES)
    rw_s, rwt = load_vec(kb, rw, KC * NE)
    id_s, idt = load_vec(kb, ident, 128)
    rb_s = kb.sb(1, F32)
    rbt = Tok('rb')
    kb.dma('sync', rb_s[0:NE, :], rb, [], [rbt], "c0")
    mv = mv_s.rearrange("p (v j k) -> p v j k", v=2, j=6)
    ae = ae_s.rearrange("p (r k j) -> p r k j", r=NCORES, j=6)
    rwv = rw_s.rearrange("p (k e) -> p k e", e=NE)
    hcur = kb.sb(KC, F32)
    hct = Tok('hcur')
    hin = [kb.sb(KC, F32) for _ in range(2)]
    hint = [Tok('hinf'), Tok('hinb')]
    for d in range(2):
        order = list(range(NCORES)) if d == 0 else list(range(NCORES - 1, -1, -1))
        kb.I('vector', 'tensor_copy', [aet], [hct], hcur, ae[:, 0, :, 4 + d])
        for ci, r in enumerate(order):
            if ci == 0:
                kb.I('vector', 'tensor_scalar', [hct, selt], [hint[d]], hin[d], hcur, sel_s[:, r:r + 1], 0.0, ALU.mult, ALU.add)
            else:
                kb.I('vector', 'scalar_tensor_tensor', [hct, selt, hint[d]], [hint[d]], hin[d], hcur, sel_s[:, r:r + 1], hin[d],
                     ALU.mult, ALU.add)
            if ci < NCORES - 1:
                kb.I('vector', 'tensor_tensor', [hct, aet], [hct], hcur, hcur, ae[:, r, :, 2 * d], ALU.mult)
                kb.I('vector', 'tensor_tensor', [hct, aet], [hct], hcur, hcur, ae[:, r, :, 2 * d + 1], ALU.add)
    m0 = kb.mark()
    mT_flat = kb.sb(KC * TL, BF16)
    mT = mT_flat.rearrange("p (k t) -> p k t", k=KC)
    mTt = Tok('mT')
    m1 = kb.mark()
    lp = [Pool(kb, nm, 2, TL, F32) for nm in ('fa', 'fb', 'fc')]
    gp = Pool(kb, 'fg', 2, TL, BF16)
    for k in range(KC):
        bufs = []
        for pl, src in zip(lp, (S0, Pf, Pb)):
            b_, t_, n_ = pl.next()
            kb.dma('sync', b_, src[k], [], [t_], "L" + n_)
            bufs.append((b_, t_))
        gb, gt, gn_ = gp.next()
        kb.dma('sync', gb, G[k], [], [gt], "L" + gn_)
        (sb_, st_), (fb_, ft_), (bb_, bt_) = bufs
        kb.I('vector', 'scalar_tensor_tensor', [ft_, st_, hint[0]], [st_], sb_, fb_, hin[0][:, k:k + 1], sb_, ALU.mult, ALU.add)
        kb.I('vector', 'scalar_tensor_tensor', [bt_, st_, hint[1]], [st_], sb_, bb_, hin[1][:, k:k + 1], sb_, ALU.mult, ALU.add)
        kb.I('vector', 'tensor_tensor', [st_, gt], [], mT[:, k, :], sb_, gb, ALU.mult, pw=[mTt])
    kb.release(m1)
    wpool = Pool(kb, 'wp', 4, KC * 128, BF16)
    xpool = Pool(kb, 'xp', 2, TL, F32)
    spool = Pool(kb, 'st', 2, TL, F32)
    acc = kb.sb(TL, F32)
    acct = Tok('acc')
    tmp = kb.sb(TL, F32)
    tmpt = Tok('tmp')
    blocks = colblocks(TL)

    def epi_out(n, tiles):
        xb, xt, xn = xpool.next()
        kb.dma('sync', xb, x1T[n * 128:(n + 1) * 128, :], [], [xt], "L" + xn)
        sb_, st_, sn_ = spool.next()
        for (ps, pt, c0, c1) in tiles:
            kb.I('vector', 'scalar_tensor_tensor', [pt, xt, mvt], [], sb_[:, c0:c1], ps, mv[:, 0, 2, n:n + 1], xb[:, c0:c1],
                 ALU.mult, ALU.add, pw=[st_])
        kb.dma('sync', outT[n * 128:(n + 1) * 128, :], sb_, [st_], [out_t[n]], "S" + sn_)
        if n == 0:
            kb.I('gpsimd', 'tensor_tensor', [st_], [acct], acc, sb_, sb_, ALU.mult)
        else:
            kb.I('scalar', 'activation', [st_], [tmpt], tmp, sb_, AF.Square)
            kb.I('gpsimd', 'tensor_tensor', [tmpt, acct], [acct], acc, acc, tmp, ALU.add)

    linear(kb, w_out, KC, list(range(KC)), lambda k, c0, c1: (mT[:, k, c0:c1], [mTt]), blocks, epi_out, wpool, [0, 1, 2, 3, 4, 5])
    kb.P.barrier()
    kb.off = m0
    rstd3 = kb.sb(TL, F32)
    rstd3t = Tok('rstd3')
    sumsq_finish(kb, acc, acct, TL, of, oft, 1.0 / D, rstd3, rstd3t, [6, 7])
    A3 = kb.sb(KC, F32)
    A3t = Tok('A3')
    kb.I('vector', 'scalar_tensor_tensor', [mvt, gn2t], [A3t], A3, mv[:, 0, 4, :], 1.0, gn2_s, ALU.add, ALU.mult)
    kb.P.barrier()
    h3_flat = kb.sb(KC * TL, BF16)
    h3 = h3_flat.rearrange("p (k t) -> p k t", k=KC)
    h3t = Tok('h3')
    m2 = kb.mark()
    xpool = Pool(kb, 'xp', 2, TL, F32)
    tpool = Pool(kb, 'tp', 2, TL, F32)
    hfp = Pool(kb, 'hf', 2, TL, F32)
    for k in range(KC):
        xb, xt, xn = xpool.next()
        kb.dma('sync', xb, outT[k * 128:(k + 1) * 128, :], [out_t[k]], [xt], "L" + xn)
        tb, tt_, _ = tpool.next()
        kb.I('vector', 'scalar_tensor_tensor', [xt, rstd3t, A3t], [tt_], tb, xb, A3[:, k:k + 1], rstd3, ALU.mult, ALU.mult)
        hb, hbt, _ = hfp.next()
        kb.I('scalar', 'activation', [tt_, mvt], [hbt], hb, tb, AF.Identity, bias=mv[:, 0, 3, k:k + 1], scale=1.0)
        kb.I('gpsimd', 'tensor_copy', [hbt], [], h3[:, k, :], hb, pw=[h3t])
        for bi, (c0, c1) in enumerate(blocks):
            kb.I('tensor', 'matmul', [hbt, rwt], [kb.pst[6 + bi]], kb.ps[6 + bi][0:NE, 0:512], lhsT=rwv[:, k, :], rhs=hb[:, c0:c1],
                 start=(k == 0), stop=(k == KC - 1))
    lg = kb.sb(TL, F32)
    lgt = Tok('lg')
    for bi, (c0, c1) in enumerate(blocks):
        kb.I('scalar', 'activation', [kb.pst[6 + bi], rbt], [], lg[0:NE, c0:c1], kb.ps[6 + bi][0:NE, 0:512], AF.Identity,
             bias=rb_s[0:NE, 0:1], scale=1.0, pw=[lgt])
    NT = TL // 128
    for t in range(NT):
        kb.I('tensor', 'matmul', [lgt, idt], [], kb.ps[0][:, t * NE:(t + 1) * NE], lhsT=lg[0:NE, t * 128:(t + 1) * 128],
             rhs=id_s[0:NE, 0:NE], start=True, stop=True, pw=[kb.pst[0]])
    L = kb.sb(NT * NE, F32)
    Lt = Tok('L')
    kb.I('vector', 'tensor_copy', [kb.pst[0]], [Lt], L, kb.ps[0][:, 0:NT * NE])
    L3 = L.rearrange("p (t e) -> p t e", e=NE)
    mx1 = kb.sb(NT, F32)
    mx2 = kb.sb(NT, F32)
    w1 = kb.sb(NT * NE, F32)
    w13 = w1.rearrange("p (t e) -> p t e", e=NE)
    w2 = kb.sb(NT * NE, F32)
    w23 = w2.rearrange("p (t e) -> p t e", e=NE)
    tk = Tok('topk')

    def bc(v):
        return v.unsqueeze(2).to_broadcast([128, NT, NE])
    kb.I('vector', 'tensor_reduce', [Lt], [tk], mx1, L3, AX.X, ALU.max)
    kb.I('vector', 'tensor_tensor', [Lt, tk], [tk], w13, L3, bc(mx1), ALU.is_equal)
    kb.I('vector', 'scalar_tensor_tensor', [Lt, tk], [tk], w13, w13, -1e30, L3, ALU.mult, ALU.add)
    kb.I('vector', 'tensor_reduce', [tk], [tk], mx2, w13, AX.X, ALU.max)
    kb.I('vector', 'tensor_tensor', [Lt, tk], [tk], w13, L3, bc(mx2), ALU.is_ge)
    msk = kb.sb(NT * NE, F32)
    kb.I('vector', 'tensor_copy', [tk], [tk], msk, w1)
    kb.I('vector', 'tensor_tensor', [Lt, tk], [tk], w23, L3, bc(mx1), ALU.subtract)
    kb.I('scalar', 'activation', [tk], [tk], w2, w2, AF.Exp)
    kb.I('vector', 'tensor_tensor', [tk], [tk], w1, w1, w2, ALU.mult)
    kb.I('vector', 'tensor_reduce', [tk], [tk], mx1, w13, AX.X, ALU.add)
    kb.I('vector', 'reciprocal', [tk], [tk], mx1, mx1)
    kb.I('vector', 'tensor_tensor', [tk], [tk], w13, w13, bc(mx1), ALU.mult)
    kb.dma('sync', comb_o, w1, [tk], [], "Scomb")
    kb.dma('sync', mask_o, msk, [tk], [], "Smask")
    for k in range(KC):
        pass
    kb.dma('sync', h3o.rearrange("k p t -> p k t"), h3, [h3t], [], "Sh3")
    return kb.finish()


def host_l1b(r1a, x1_lat, mod1, norm_ffn, w_out, router_w, router_b):
    AEa = np.ascontiguousarray(np.concatenate([r1a[i]["AE"].reshape(128, 1, KC * 6) for i in range(NCORES)], axis=1).reshape(128, -1))
    xTf = np.ascontiguousarray(x1_lat.T)
    rwf = np.ascontiguousarray(np.moveaxis(router_w.reshape(KC, 128, NE), 1, 0).reshape(128, KC * NE))
    common = dict(host_consts(), AEa=AEa, modv=modv_layout(mod1), gn2=fm(norm_ffn), w_out=w_out, rw=rwf,
                  rb=np.ascontiguousarray(router_b.reshape(NE, 1)), ident=np.eye(128, dtype=np.float32))
    in_maps = []
    for i in range(NCORES):
        m = dict(common)
        for nm in ("S0", "Pf", "Pb", "G"):
            m[nm] = np.ascontiguousarray(r1a[i][nm])
        s = np.zeros((128, NCORES), np.float32)
        s[:, i] = 1.0
        m["sel"] = s
        m["x1T"] = np.ascontiguousarray(xTf[:, i * TL:(i + 1) * TL])
        in_maps.append(m)
    return run("l1b", build_l1b, in_maps)


def build_l1c(Cp):
    kb = KB()
    hg = kb.din("hg", [KC, 128, Cp], BF16)
    wrow = kb.din("wrow", [128, Cp])
    g2 = kb.din("g2", [128, KC])
    ewg = kb.din("ewg", [D, D])
    ewu = kb.din("ewu", [D, D])
    ewd = kb.din("ewd", [D, D])
    yT = kb.dout("yT", [KC, 128, Cp])
    g2_s, g2t = load_vec(kb, g2, KC)
    m0 = kb.mark()
    for (t0, t1) in colblocks(Cp, 1024):
        Tt = t1 - t0
        blocks = colblocks(Tt)
        nb = len(blocks)
        hT_flat = kb.sb(KC * Tt, BF16)
        hT = hT_flat.rearrange("p (k t) -> p k t", k=KC)
        ht = Tok('hT')
        for k in range(KC):
            kb.dma('sync', hT[:, k, :], hg[k][:, t0:t1], [], [], f"Lh{k % 2}", pw=[ht])
        wb = kb.sb(Tt, F32)
        wbt = Tok('wb')
        kb.dma('sync', wb, wrow[:, t0:t1], [], [wbt], "Lwb")
        aT_flat = kb.sb(KC * Tt, BF16)
        aT = aT_flat.rearrange("p (f t) -> p f t", f=KC)
        aTt = Tok('aT')
        wpool = Pool(kb, 'wp', 4, KC * 128, BF16)
        sgp = Pool(kb, 'sg', 2, Tt, BF16)
        tgp = Pool(kb, 'tg', 2, Tt, BF16)
        spool = Pool(kb, 'so', 2, Tt, F32)
        pit = 0
        for f in range(KC):
            bs = (pit % 2) * 4
            pit += 1
            for wi, W in enumerate((ewg, ewu)):
                wbuf, wt, wn = wpool.next()
                wv = wbuf.rearrange("p (k n) -> p k n", n=128)
                kb.dma('gpsimd', wv, W[:, f * 128:(f + 1) * 128].rearrange("(k p) n -> p k n", p=128), [], [wt], "L" + wn)
                for k in range(KC):
                    for bi, (c0, c1) in enumerate(blocks):
                        b = bs + wi * 2 + bi
                        kb.I('tensor', 'matmul', [wt, ht], [kb.pst[b]], kb.ps[b][:, 0:c1 - c0], lhsT=wv[:, k, :], rhs=hT[:, k, c0:c1],
                             start=(k == 0), stop=(k == KC - 1))
            sg, sgt, _ = sgp.next()
            tg, tgt, _ = tgp.next()
            for bi, (c0, c1) in enumerate(blocks):
                kb.I('scalar', 'activation', [kb.pst[bs + bi]], [], sg[:, c0:c1], kb.ps[bs + bi][:, 0:c1 - c0], AF.Silu, pw=[sgt])
            for bi, (c0, c1) in enumerate(blocks):
                kb.I('vector', 'tensor_tensor', [sgt, kb.pst[bs + 2 + bi]], [], tg[:, c0:c1], sg[:, c0:c1], kb.ps[bs + 2 + bi][:, 0:c1 - c0],
                     ALU.mult, pw=[tgt])
            kb.I('gpsimd', 'tensor_tensor', [tgt, wbt], [], aT[:, f, :], tg, wb, ALU.mult, pw=[aTt])

        def epi_dn(n, tiles, spool=spool, t0=t0, t1=t1):
            sb_, st_, sn_ = spool.next()
            for (ps, pt, c0, c1) in tiles:
                kb.I('scalar', 'activation', [pt, g2t], [], sb_[:, c0:c1], ps, AF.Identity, scale=g2_s[:, n:n + 1], bias=0.0, pw=[st_])
            kb.dma('sync', yT[n][:, t0:t1], sb_, [st_], [], "S" + sn_)

        banks = [0, 1, 2, 3, 4, 5, 6, 7] if nb == 2 else [0, 1, 2, 3, 4, 5, 6, 7][:8 // nb * nb]
        linear(kb, ewd, KC, list(range(KC)), lambda k, c0, c1, aT=aT, aTt=aTt: (aT[:, k, c0:c1], [aTt]), blocks, epi_dn, wpool, banks)
        kb.release(m0)
    return kb.finish()


def build_l1d():
    kb = KB()
    x2T = kb.din("x2T", [D, TL])
    ya = kb.din("ya", [KC, 128, TL])
    yb = kb.din("yb", [KC, 128, TL])
    outT = kb.dout("outT", [D, TL])
    pa = Pool(kb, 'da', 2, TL, F32)
    pb = Pool(kb, 'db', 2, TL, F32)
    px = Pool(kb, 'dx', 2, TL, F32)
    for n in range(KC):
        xb, xt, xn = px.next()
        ab, at, an = pa.next()
        bb, bt, bn = pb.next()
        kb.dma('sync', xb, x2T[n * 128:(n + 1) * 128, :], [], [xt], "L" + xn)
        kb.dma('sync', ab, ya[n], [], [at], "L" + an)
        kb.dma('sync', bb, yb[n], [], [bt], "L" + bn)
        kb.I('vector', 'tensor_tensor', [at, bt], [at], ab, ab, bb, ALU.add)
        kb.I('vector', 'tensor_tensor', [at, xt], [xt], xb, xb, ab, ALU.add)
        kb.dma('sync', outT[n * 128:(n + 1) * 128, :], xb, [xt], [], "S" + xn)
    return kb.finish()


def host_moe(r1b, g2_lat, ewg, ewu, ewd):
    NT = TL // 128
    def tokmajor(a):
        return a.reshape(128, NT, NE).transpose(1, 0, 2).reshape(TL, NE)
    comb = np.concatenate([tokmajor(r1b[i]["comb"]) for i in range(NCORES)], axis=0)
    mask = np.concatenate([tokmajor(r1b[i]["mask"]) for i in range(NCORES)], axis=0) > 0.5
    h3 = np.concatenate([r1b[i]["h3T"].reshape(D, TL) for i in range(NCORES)], axis=1)
    idx = [np.nonzero(mask[:, e])[0] for e in range(NE)]
    nmax = max(len(ix) for ix in idx)
    Cp = max(512, -(-nmax // 512) * 512)
    in_maps = []
    for e in range(NE):
        ne = len(idx[e])
        hg = np.zeros((D, Cp), NPBF)
        hg[:, :ne] = h3[:, idx[e]]
        wr = np.zeros((128, Cp), np.float32)
        wr[:, :ne] = comb[idx[e], e][None, :]
        in_maps.append({"hg": hg.reshape(KC, 128, Cp), "wrow": wr, "g2": g2_lat, "ewg": np.ascontiguousarray(ewg[e]),
                        "ewu": np.ascontiguousarray(ewu[e]), "ewd": np.ascontiguousarray(ewd[e])})
    rc = run(f"l1c_{Cp}", lambda: build_l1c(Cp), in_maps)
    rank = np.cumsum(mask, axis=1) - 1
    Ya = np.zeros((D, S), np.float32)
    Yb = np.zeros((D, S), np.float32)
    for e in range(NE):
        ne = len(idx[e])
        y = rc[e]["yT"].reshape(D, Cp)[:, :ne]
        sl = rank[idx[e], e]
        Ya[:, idx[e][sl == 0]] = y[:, sl == 0]
        Yb[:, idx[e][sl >= 1]] = y[:, sl >= 1]
    in_maps = []
    for i in range(NCORES):
        in_maps.append({"x2T": np.ascontiguousarray(r1b[i]["outT"]),
                        "ya": np.ascontiguousarray(Ya[:, i * TL:(i + 1) * TL]).reshape(KC, 128, TL),
                        "yb": np.ascontiguousarray(Yb[:, i * TL:(i + 1) * TL]).reshape(KC, 128, TL)})
    return run("l1d", build_l1d, in_maps)


def kernel(**I):
    g = lambda k: np.asarray(I[k])
    mod = host_mod(g('c'), g('c_ctx'), g('attn_w_mod')[0], g('attn_b_mod')[0], g('rec_w_mod')[0], g('rec_b_mod')[0])
    x = g('x')[0]
    ctx = g('ctx')[0]
    r0a = host_l0a(x, ctx, mod[0], g('attn_norm_mix')[0], g('attn_w_in')[0], g('attn_gqa_q_norm')[0], g('attn_gqa_k_norm')[0],
                   g('attn_diff_q_norm')[0], g('attn_diff_k_norm')[0])
    r0b1 = host_l0b1(r0a, g('attn_diff_lambda_q1')[0], g('attn_diff_lambda_k1')[0], g('attn_diff_lambda_q2')[0],
                     g('attn_diff_lambda_k2')[0], g('attn_diff_subln')[0])
    del r0a
    r0b2 = host_l0b2(r0b1, x, ctx, mod[0], g('attn_norm_ffn')[0], g('attn_w_out')[0], g('ffn_w_gate')[0], g('ffn_w_up')[0],
                     g('ffn_w_down')[0])
    del r0b1
    x1 = np.ascontiguousarray(np.concatenate([r0b2[i]["x1T"][:, 0:TL] for i in range(NCORES)], axis=1).T)
    ctx1 = np.ascontiguousarray(r0b2[0]["x1T"][:, TL:T0].T)
    del r0b2
    r1a = host_l1a(x1, ctx1, mod[1], g('rec_norm_mix')[0], g('rec_w_in')[0], g('rec_conv_w')[0], g('rec_conv_b')[0],
                   g('rec_gate_a_w')[0], g('rec_gate_a_b')[0], g('rec_gate_x_w')[0], g('rec_gate_x_b')[0], g('rec_lru_lambda')[0])
    r1b = host_l1b(r1a, x1, mod[1], g('rec_norm_ffn')[0], g('rec_w_out')[0], g('moe_router_w')[0], g('moe_router_b')[0])
    del r1a
    g2_lat = fm(mod[1][0, 5])
    r1d = host_moe(r1b, g2_lat, g('moe_w_gate')[0], g('moe_w_up')[0], g('moe_w_down')[0])
    out = np.concatenate([r1d[i]["outT"] for i in range(NCORES)], axis=1).T
    return np.ascontiguousarray(out.reshape(1, S, D).astype(np.float32))
```

```python
import numpy as np
from contextlib import ExitStack
import ml_dtypes
import concourse.bass as bass
import concourse.mybir as mybir
from concourse.bass_utils import run_bass_kernel_spmd

F32 = mybir.dt.float32
BF16 = mybir.dt.bfloat16
AF = mybir.ActivationFunctionType
ALU = mybir.AluOpType
AX = mybir.AxisListType
NPBF = ml_dtypes.bfloat16

NCORES = 8
D = 4096
KC = 32
S = 8192
TL = 1024
TC = 256
EPS = 1e-6
ENGINES = ['sync', 'scalar', 'vector', 'gpsimd', 'tensor']
ARENA = 102400


class Tok:
    __slots__ = ('name', 'writers', 'readers', 'pending_war')

    def __init__(self, name=''):
        self.name = name
        self.writers = []
        self.readers = []
        self.pending_war = []


class Op:
    __slots__ = ('eng', 'fn', 'deps', 'dma_sem', 'idx', 'signal', 'sem', 'val')


class Prog:
    def __init__(self, nc):
        self.nc = nc
        self.ops = []
        self.eng_ops = {e: [] for e in ENGINES}

    def op(self, eng, fn, reads=(), writes=(), dma=None, pwrites=()):
        o = Op()
        o.eng = eng
        o.fn = fn
        o.deps = set()
        o.dma_sem = dma
        o.signal = False
        o.sem = None
        o.val = 0
        o.idx = len(self.ops)
        for t in reads:
            for w in t.writers:
                o.deps.add(w)
            t.readers.append(o)
        for t in writes:
            self._write(o, t, True)
        for t in pwrites:
            self._write(o, t, False)
        o.deps.discard(o)
        self.ops.append(o)
        self.eng_ops[eng].append(o)
        return o

    def _write(self, o, t, exclusive):
        if t.readers:
            for r in t.readers:
                o.deps.add(r)
            t.pending_war = t.readers
            t.readers = []
            if exclusive:
                for w in t.writers:
                    o.deps.add(w)
            t.writers = [o]
        else:
            for r in t.pending_war:
                o.deps.add(r)
            if exclusive:
                for w in t.writers:
                    o.deps.add(w)
                t.writers = [o]
            else:
                t.writers.append(o)

    def barrier(self):
        last = []
        for e in ENGINES:
            if self.eng_ops[e]:
                for o in reversed(self.eng_ops[e]):
                    if o.fn is not None and o.dma_sem is None:
                        last.append(o)
                        break
        seen = {}
        for o in self.ops:
            if o.dma_sem is not None:
                seen[o.dma_sem] = o
        last.extend(seen.values())
        for e in ENGINES:
            b = self.op(e, None)
            for o in last:
                b.deps.add(o)

    def emit(self, stack):
        nc = self.nc
        for o in self.ops:
            for d in o.deps:
                if d.eng == 'tensor' and o.eng == 'tensor' and d.dma_sem is None:
                    continue
                d.signal = True
        eng_sem = {e: stack.enter_context(nc.semaphore('s_' + e)) for e in ENGINES}
        dsem = {}
        dcount = {}
        for o in self.ops:
            if o.dma_sem is not None and o.dma_sem not in dsem:
                dsem[o.dma_sem] = stack.enter_context(nc.semaphore('d_' + o.dma_sem))
                dcount[o.dma_sem] = 0
        ecount = {e: 0 for e in ENGINES}
        for o in self.ops:
            if o.fn is None:
                continue
            if o.dma_sem is not None:
                dcount[o.dma_sem] += 16
                o.sem = dsem[o.dma_sem]
                o.val = dcount[o.dma_sem]
                o.signal = True
            elif o.signal:
                ecount[o.eng] += 1
                o.sem = eng_sem[o.eng]
                o.val = ecount[o.eng]
        self.n_sems = len(eng_sem) + len(dsem)
        block = stack.enter_context(nc.Block())
        prog = self

        def make(ename):
            def body(eng):
                waited = {}
                for o in prog.eng_ops[ename]:
                    need = {}
                    for d in o.deps:
                        if d.fn is None:
                            continue
                        if d.eng == 'tensor' and ename == 'tensor' and d.dma_sem is None:
                            continue
                        k = id(d.sem)
                        if k not in need or need[k][1] < d.val:
                            need[k] = (d.sem, d.val)
                    for k, (s, v) in need.items():
                        if waited.get(k, 0) >= v:
                            continue
                        eng.wait_ge(s, v)
                        waited[k] = v
                    if o.fn is None:
                        continue
                    ins = o.fn(eng)
                    if o.signal:
                        ins.then_inc(o.sem, 16 if o.dma_sem is not None else 1)
                if ename == 'sync':
                    for nm, s in dsem.items():
                        eng.wait_ge(s, dcount[nm])
            return body

        block.sync(make('sync'))
        block.scalar(make('scalar'))
        block.vector(make('vector'))
        block.gpsimd(make('gpsimd'))
        block.tensor(make('tensor'))


class Pool:
    def __init__(self, kb, name, n, cols, dt):
        self.bufs = [kb.sb(cols, dt) for _ in range(n)]
        self.toks = [Tok(f"{name}{i}") for i in range(n)]
        self.i = 0
        self.name = name
        self.n = n

    def next(self):
        i = self.i % self.n
        self.i += 1
        return self.bufs[i], self.toks[i], f"{self.name}{i}"


class KB:
    def __init__(self):
        self.nc = bass.Bass("TRN2", target_bir_lowering=False)
        nc = self.nc
        self.P = Prog(nc)
        self.st = ExitStack()
        self.arena = self.st.enter_context(nc.sbuf_tensor("arena", [128, ARENA], BF16))
        self.off = 0
        self.ps = [self.st.enter_context(nc.psum_tensor(f"ps{i}", [128, 512], F32)) for i in range(8)]
        self.pst = [Tok(f"ps{i}") for i in range(8)]
        self.outs = []

    def din(self, name, shape, dt=F32):
        return self.nc.dram_tensor(name, list(shape), dt, kind="ExternalInput").ap()

    def dout(self, name, shape, dt=F32):
        self.outs.append(name)
        return self.nc.dram_tensor(name, list(shape), dt, kind="ExternalOutput").ap()

    def dscr(self, name, shape, dt=F32):
        return self.nc.dram_tensor(name, list(shape), dt, kind="Internal").ap()

    def sb(self, cols, dt=F32):
        n16 = cols * (2 if dt == F32 else 1)
        n16 += n16 % 2
        off = self.off
        self.off += n16
        assert self.off <= ARENA, f"SBUF arena overflow {self.off}"
        ap = self.arena[:, off:off + n16]
        if dt == F32:
            ap = ap.bitcast(F32)
        return ap

    def mark(self):
        return self.off

    def release(self, mark):
        self.P.barrier()
        self.off = mark

    def I(self, eng, meth, r, w, *a, pw=(), **kw):
        return self.P.op(eng, lambda e: getattr(e, meth)(*a, **kw), reads=r, writes=w, pwrites=pw)

    def dma(self, q, out, in_, r, w, grp, pw=()):
        return self.P.op(q, lambda e: e.dma_start(out=out, in_=in_), reads=r, writes=w, pwrites=pw, dma=grp)

    def finish(self):
        self.P.emit(self.st)
        self.st.close()
        return self.nc


def colblocks(T, bs=512):
    return [(c, min(c + bs, T)) for c in range(0, T, bs)]


def load_vec(kb, dram_ap, cols, q='sync', name='v'):
    t = kb.sb(cols, F32)
    tok = Tok(name)
    kb.nd = getattr(kb, 'nd', 0) + 1
    kb.dma(q, t, dram_ap, [], [tok], f"c{kb.nd % 4}")
    return t, tok


def linear(kb, W, nK, n_list, rhs_fn, blocks, epi, wpool, banks, kg=32, wcol0=0):
    nb = len(blocks)
    nset = len(banks) // nb
    it = getattr(kb, '_lin_it', 0)
    for n in n_list:
        bset = banks[(it % nset) * nb:(it % nset + 1) * nb]
        it += 1
        for k0 in range(0, nK, kg):
            k1 = min(k0 + kg, nK)
            wb, wt, wn = wpool.next()
            wv = wb[:, 0:(k1 - k0) * 128].rearrange("p (k n) -> p k n", n=128)
            src = W[k0 * 128:k1 * 128, wcol0 + n * 128:wcol0 + (n + 1) * 128].rearrange("(k p) n -> p k n", p=128)
            kb.dma('gpsimd', wv, src, [], [wt], "L" + wn)
            for k in range(k0, k1):
                for bi, (c0, c1) in enumerate(blocks):
                    rap, rtoks = rhs_fn(k, c0, c1)
                    b = bset[bi]
                    kb.I('tensor', 'matmul', [wt] + rtoks, [kb.pst[b]], kb.ps[b][:, 0:c1 - c0],
                         lhsT=wv[:, k - k0, :], rhs=rap, start=(k == 0), stop=(k == nK - 1))
        epi(n, [(kb.ps[bset[bi]][:, 0:c1 - c0], kb.pst[bset[bi]], c0, c1) for bi, (c0, c1) in enumerate(blocks)])
    kb._lin_it = it


def sumsq_finish(kb, acc, acc_tok, T, ones_f32, ones_tok, inv_n, out, out_tok, banks):
    for bi, (c0, c1) in enumerate(colblocks(T)):
        b = banks[bi % len(banks)]
        kb.I('tensor', 'matmul', [acc_tok, ones_tok], [kb.pst[b]], kb.ps[b][:, 0:c1 - c0],
             lhsT=ones_f32, rhs=acc[:, c0:c1], start=True, stop=True)
        kb.I('vector', 'tensor_scalar', [kb.pst[b]], [], out[:, c0:c1], kb.ps[b][:, 0:c1 - c0],
             inv_n, EPS, ALU.mult, ALU.add, pw=[out_tok])
    kb.I('scalar', 'activation', [out_tok], [out_tok], out[:, 0:T], out[:, 0:T], AF.Sqrt)
    kb.I('vector', 'reciprocal', [out_tok], [out_tok], out[:, 0:T], out[:, 0:T])


def modulate(kb, src_fn, T, A, B, vtoks, hT, h_tok, ones_f32, ones_tok, banks, xpool, hcol0=0, segs=None):
    if segs is None:
        segs = [(0, T, 0)]
    acc = kb.sb(T, F32)
    acc_t = Tok('acc')
    rstd = kb.sb(T, F32)
    rstd_t = Tok('rstd')
    tmp_pool = Pool(kb, 'mt', 2, T, F32)
    for k in range(KC):
        xb, xt, xn = xpool.next()
        kb.dma('sync', xb[:, 0:T], src_fn(k), [], [xt], "L" + xn)
        if k == 0:
            kb.I('vector', 'tensor_tensor', [xt], [acc_t], acc, xb[:, 0:T], xb[:, 0:T], ALU.mult)
        else:
            tb, tt, _ = tmp_pool.next()
            kb.I('scalar', 'activation', [xt], [tt], tb, xb[:, 0:T], AF.Square)
            kb.I('vector', 'tensor_tensor', [tt, acc_t], [acc_t], acc, acc, tb, ALU.add)
    sumsq_finish(kb, acc, acc_t, T, ones_f32, ones_tok, 1.0 / D, rstd, rstd_t, banks)
    for k in range(KC):
        xb, xt, xn = xpool.next()
        kb.dma('sync', xb[:, 0:T], src_fn(k), [], [xt], "L" + xn)
        tb, tt, _ = tmp_pool.next()
        for (c0, c1, vi) in segs:
            kb.I('vector', 'scalar_tensor_tensor', [xt, rstd_t] + vtoks, [], tb[:, c0:c1], xb[:, c0:c1], A[vi][:, k:k + 1],
                 rstd[:, c0:c1], ALU.mult, ALU.mult, pw=[tt])
        for (c0, c1, vi) in segs:
            kb.I('scalar', 'activation', [tt] + vtoks, [], hT[:, k, hcol0 + c0:hcol0 + c1], tb[:, c0:c1], AF.Identity,
                 bias=B[vi][:, k:k + 1], scale=1.0, pw=[h_tok])


def mod_vectors(kb, modv, gn, vtok):
    mv = modv.rearrange("p (v j k) -> p v j k", v=2, j=6)
    return mv


MODW = 6 * D // NCORES


def build_mod():
    kb = KB()
    cv = kb.din("cv", [128, KC * 2])
    w0 = kb.din("w0", [D, MODW])
    w1 = kb.din("w1", [D, MODW])
    b0 = kb.din("b0", [1, MODW])
    b1 = kb.din("b1", [1, MODW])
    out = kb.dout("modo", [2, 2, MODW])
    cs, ct = load_vec(kb, cv, KC * 2)
    sc = kb.sb(KC * 2, F32)
    sct = Tok('sc')
    kb.I('scalar', 'activation', [ct], [sct], sc, cs, AF.Silu)
    scv = sc.rearrange("p (k v) -> p k v", v=2)
    wpool = Pool(kb, 'mw', 2, KC * 512 * 2, BF16)
    res = kb.sb(MODW, F32)
    bia = kb.sb(MODW, F32)
    bank_i = 0
    for li, (w, b) in enumerate([(w0, b0), (w1, b1)]):
        bt = Tok('bias')
        rt = Tok('res')
        kb.dma('sync', bia[0:1, :], b, [], [], "bia", pw=[bt])
        kb.dma('sync', bia[1:2, :], b, [], [], "bia", pw=[bt])
        for cb in range(MODW // 512):
            wb, wt, wn = wpool.next()
            wv = wb.bitcast(F32).rearrange("p (k n) -> p k n", n=512)
            kb.dma('sync' if cb % 2 == 0 else 'scalar', wv,
                   w[:, cb * 512:(cb + 1) * 512].rearrange("(k p) n -> p k n", p=128), [], [wt], "L" + wn)
            bk = bank_i % 4
            bank_i += 1
            for k in range(KC):
                kb.I('tensor', 'matmul', [wt, sct], [kb.pst[bk]], kb.ps[bk][0:2, 0:512],
                     lhsT=scv[:, k, :], rhs=wv[:, k, :], start=(k == 0), stop=(k == KC - 1))
            kb.I('vector', 'tensor_tensor', [kb.pst[bk], bt], [], res[0:2, cb * 512:(cb + 1) * 512],
                 kb.ps[bk][0:2, 0:512], bia[0:2, cb * 512:(cb + 1) * 512], ALU.add, pw=[rt])
        kb.dma('sync', out[li], res[0:2, :], [rt], [], "out")
    return kb.finish()


_CACHE = {}


def get_prog(name, fn):
    if name not in _CACHE:
        _CACHE[name] = fn()
    return _CACHE[name]


def run(name, fn, in_maps):
    import time as _t
    t0 = _t.time()
    nc = get_prog(name, fn)
    t1 = _t.time()
    res = run_bass_kernel_spmd(nc, in_maps, core_ids=list(range(NCORES)))
    print(f"[launch {name}] build {t1 - t0:.1f}s run {_t.time() - t1:.1f}s", flush=True)
    return res.results


def fm(v):
    v = np.asarray(v, np.float32)
    lead = v.shape[:-1]
    return np.ascontiguousarray(np.moveaxis(v.reshape(lead + (KC, 128)), -1, 0))


def host_mod(c, c_ctx, w_mod0, b_mod0, w_mod1, b_mod1):
    cv = np.ascontiguousarray(np.stack([fm(c.reshape(-1)), fm(c_ctx.reshape(-1))], axis=-1).reshape(128, KC * 2))
    in_maps = []
    for i in range(NCORES):
        sl = slice(i * MODW, (i + 1) * MODW)
        in_maps.append({"cv": cv, "w0": np.ascontiguousarray(w_mod0[:, sl]), "w1": np.ascontiguousarray(w_mod1[:, sl]),
                        "b0": np.ascontiguousarray(b_mod0[sl].reshape(1, MODW)), "b1": np.ascontiguousarray(b_mod1[sl].reshape(1, MODW))})
    r = run("mod", build_mod, in_maps)
    full = np.concatenate([r[i]["modo"] for i in range(NCORES)], axis=-1)
    return full.reshape(2, 2, 6, D)


T0 = TL + TC
NQ = 32
NKCH = 20
VW = 2560


def consts_common(kb):
    c = {}
    c['ones_f'] = kb.din("ones_f", [128, 128])
    c['ones_b'] = kb.din("ones_b", [128, 128], BF16)
    of, oft = load_vec(kb, c['ones_f'], 128)
    ob = kb.sb(128, BF16)
    obt = Tok('ones_b')
    kb.dma('sync', ob, c['ones_b'], [], [obt], "cb")
    return of, oft, ob, obt


def host_consts():
    return {"ones_f": np.ones((128, 128), np.float32), "ones_b": np.ones((128, 128), NPBF)}


def build_l0a():
    kb = KB()
    xT = kb.din("xT", [D, T0])
    modv = kb.din("modv", [128, 2 * 6 * KC])
    gn = kb.din("gn", [128, KC])
    w_in = kb.din("w_in", [D, 9216])
    qkn = kb.din("qkn", [128, 6])
    cos = kb.din("cos", [128, TL])
    sin = kb.din("sin", [128, TL])
    rmat = kb.din("rmat", [128, 128], BF16)
    qT = kb.dout("qT", [NQ, 128, T0], BF16)
    kT = kb.dout("kT", [NKCH, 128, T0], BF16)
    Vo = kb.dout("V", [T0, VW], BF16)
    of, oft, ob, obt = consts_common(kb)
    mv_s, mvt = load_vec(kb, modv, 2 * 6 * KC)
    gn_s, gnt = load_vec(kb, gn, KC)
    qkn_s, qknt = load_vec(kb, qkn, 6)
    cos_s, cost = load_vec(kb, cos, TL)
    sin_s, sint = load_vec(kb, sin, TL)
    rm = kb.sb(128, BF16)
    rmt = Tok('rmat')
    kb.dma('sync', rm, rmat, [], [rmt], "cb")
    mv = mv_s.rearrange("p (v j k) -> p v j k", v=2, j=6)
    A = [kb.sb(KC, F32) for _ in range(2)]
    At = Tok('A')
    for v in range(2):
        kb.I('vector', 'scalar_tensor_tensor', [mvt, gnt], [], A[v], mv[:, v, 1, :], 1.0, gn_s, ALU.add, ALU.mult, pw=[At])
    Bv = [mv[:, v, 0, :] for v in range(2)]
    geff = kb.sb(6, F32)
    gefft = Tok('geff')
    kb.I('vector', 'tensor_copy', [qknt], [gefft], geff, qkn_s)
    for gi in (0, 2, 3):
        kb.I('vector', 'tensor_scalar', [gefft], [gefft], geff[:, gi:gi + 1], geff[:, gi:gi + 1], 128.0 ** -0.5, 0.0, ALU.mult, ALU.add)
    hT_flat = kb.sb(KC * T0, BF16)
    hT = hT_flat.rearrange("p (k t) -> p k t", k=KC)
    ht = Tok('hT')
    m0 = kb.mark()
    xpool = Pool(kb, 'xp', 2, T0, F32)
    modulate(kb, lambda k: xT[k * 128:(k + 1) * 128, :], T0, A, Bv, [At, mvt], hT, ht, of, oft, [6, 7], xpool,
             segs=[(0, TL, 0), (TL, T0, 1)])
    kb.release(m0)
    wpool = Pool(kb, 'wp', 4, KC * 128, BF16)
    upool = Pool(kb, 'u', 2, T0, F32)
    sq = kb.sb(T0, F32)
    sqt = Tok('sq')
    rstd = kb.sb(T0, F32)
    rstdt = Tok('rstd')
    unpool = Pool(kb, 'un', 2, T0, BF16)
    t1 = kb.sb(TL, F32)
    t1t = Tok('t1')
    t2 = kb.sb(TL, F32)
    t2t = Tok('t2')
    opool = Pool(kb, 'o', 2, TL, BF16)
    blocks = colblocks(T0)
    state = {'i': 0}

    def epi(n, tiles):
        if n < 16:
            gi, dst = 0, qT[n]
        elif n < 20:
            gi, dst = 1, kT[n - 16]
        elif n < 40:
            gi, dst = 2 + (n - 24) % 2, qT[16 + n - 24]
        else:
            gi, dst = 4 + (n - 40) % 2, kT[4 + n - 40]
        ub, ut, _ = upool.next()
        for (ps, pt, c0, c1) in tiles:
            kb.I('scalar', 'activation', [pt], [], ub[:, c0:c1], ps, AF.Copy, pw=[ut])
            kb.I('scalar', 'activation', [pt], [], sq[:, c0:c1], ps, AF.Square, pw=[sqt])
        for bi, (c0, c1) in enumerate(blocks):
            b = 6 + (state['i'] % 2)
            state['i'] += 1
            kb.I('tensor', 'matmul', [sqt, oft], [kb.pst[b]], kb.ps[b][:, 0:c1 - c0], lhsT=of, rhs=sq[:, c0:c1], start=True, stop=True)
            kb.I('vector', 'tensor_scalar', [kb.pst[b]], [], rstd[:, c0:c1], kb.ps[b][:, 0:c1 - c0], 1.0 / 128, EPS, ALU.mult, ALU.add,
                 pw=[rstdt])
        kb.I('scalar', 'activation', [rstdt], [rstdt], rstd, rstd, AF.Sqrt)
        kb.I('vector', 'reciprocal', [rstdt], [rstdt], rstd, rstd)
        unb, unt, unn = unpool.next()
        kb.I('vector', 'scalar_tensor_tensor', [ut, rstdt, gefft], [unt], unb, ub, geff[:, gi:gi + 1], rstd, ALU.mult, ALU.mult)
        obuf, otk, on = opool.next()
        kb.I('gpsimd', 'tensor_tensor', [unt, cost], [t1t], t1, unb[:, 0:TL], cos_s, ALU.mult)
        for bi, (c0, c1) in enumerate(colblocks(TL)):
            b = 6 + (state['i'] % 2)
            state['i'] += 1
            kb.I('tensor', 'matmul', [unt, rmt], [kb.pst[b]], kb.ps[b][:, 0:c1 - c0], lhsT=rm, rhs=unb[:, c0:c1], start=True, stop=True)
            kb.I('vector', 'tensor_tensor', [kb.pst[b], sint], [], t2[:, c0:c1], kb.ps[b][:, 0:c1 - c0], sin_s[:, c0:c1], ALU.mult, pw=[t2t])
        kb.I('vector', 'tensor_tensor', [t1t, t2t], [otk], obuf, t1, t2, ALU.add)
        kb.dma('sync', dst[:, 0:TL], obuf, [otk], [], "S" + on)
        kb.dma('sync', dst[:, TL:T0], unb[:, TL:T0], [unt], [], "S" + unn)

    n_list = list(range(0, 20)) + list(range(24, 56))
    linear(kb, w_in, KC, n_list, lambda k, c0, c1: (hT[:, k, c0:c1], [ht]), blocks, epi, wpool, [0, 1, 2, 3, 4, 5])
    kb.release(m0)
    vw = Pool(kb, 'vw', 2, KC * 512, BF16)
    vo = Pool(kb, 'vo', 3, 512, BF16)
    it = 0
    for cg in range(5):
        wcol = 2560 if cg == 0 else 7168 + (cg - 1) * 512
        wb, wt, wn = vw.next()
        wv = wb.rearrange("p (k n) -> p k n", n=512)
        kb.dma('gpsimd', wv, w_in[:, wcol:wcol + 512].rearrange("(k p) n -> p k n", p=128), [], [wt], "L" + wn)
        for tt in range(T0 // 128):
            b = it % 6
            it += 1
            for k in range(KC):
                kb.I('tensor', 'matmul', [wt, ht], [kb.pst[b]], kb.ps[b][:, 0:512], lhsT=hT[:, k, tt * 128:(tt + 1) * 128],
                     rhs=wv[:, k, :], start=(k == 0), stop=(k == KC - 1))
            vb, vt, vn = vo.next()
            if it % 2 == 0:
                kb.I('scalar', 'activation', [kb.pst[b]], [vt], vb, kb.ps[b][:, 0:512], AF.Copy)
            else:
                kb.I('vector', 'tensor_copy', [kb.pst[b]], [vt], vb, kb.ps[b][:, 0:512])
            kb.dma('sync', Vo[tt * 128:(tt + 1) * 128, cg * 512:(cg + 1) * 512], vb, [vt], [], "S" + vn)
    return kb.finish()


def rope_tables(core):
    half = 64
    inv = 10000.0 ** (-np.arange(0, half, 2, dtype=np.float32) / half)
    tok = np.arange(core * TL, (core + 1) * TL)
    row = (tok // 64).astype(np.float32)
    col = (tok % 64).astype(np.float32)
    ang = np.concatenate([row[:, None] * inv, col[:, None] * inv], axis=-1)
    ang = np.repeat(ang, 2, axis=1).T
    return np.ascontiguousarray(np.cos(ang).astype(np.float32)), np.ascontiguousarray(np.sin(ang).astype(np.float32))


def rot_matrix():
    r = np.zeros((128, 128), np.float32)
    for i in range(64):
        r[2 * i + 1, 2 * i] = -1.0
        r[2 * i, 2 * i + 1] = 1.0
    return r.astype(NPBF)


def modv_layout(mod_l):
    return np.ascontiguousarray(fm(mod_l).reshape(128, 2 * 6 * KC))


def host_l0a(x, ctx, mod0, norm_mix, w_in, qn_a, kn_a, qn_b, kn_b):
    xTf = np.ascontiguousarray(x.T)
    cT = np.ascontiguousarray(ctx.T)
    qkn = np.ascontiguousarray(np.stack([qn_a, kn_a, qn_b[0], qn_b[1], kn_b[0], kn_b[1]], axis=1).astype(np.float32))
    common = dict(host_consts(), modv=modv_layout(mod0), gn=fm(norm_mix), w_in=w_in, qkn=qkn, rmat=rot_matrix())
    in_maps = []
    for i in range(NCORES):
        cs, sn = rope_tables(i)
        m = dict(common)
        m["xT"] = np.ascontiguousarray(np.concatenate([xTf[:, i * TL:(i + 1) * TL], cT], axis=1))
        m["cos"] = cs
        m["sin"] = sn
        in_maps.append(m)
    return run("l0a", build_l0a, in_maps)


NKEY = TC + S
NKT = NKEY // 128
DFF = 11008
NF = DFF // 128
LAM_INIT0 = 0.8 - 0.6 * float(np.exp(-0.3 * 0))


def attn_unit(kb, q_ap, qtok, kTs, ktok, Vs, vtok, ndv, outs, outtok, ob, obt, ptpool, rz, rzt):
    SB = [0, 1, 2]
    OB = [3, 4]
    ZB = 5
    for (c0, c1, nkt) in [(0, 512, NKT), (512, 1024, NKT), (1024, 1280, 2)]:
        w = c1 - c0

        def s_mm(kt):
            b = SB[kt % 3]
            kb.I('tensor', 'matmul', [ktok, qtok], [kb.pst[b]], kb.ps[b][:, 0:w], lhsT=kTs[:, kt * 128:(kt + 1) * 128],
                 rhs=q_ap[:, c0:c1], start=True, stop=True)
        s_mm(0)
        for kt in range(nkt):
            if kt + 1 < nkt:
                s_mm(kt + 1)
            b = SB[kt % 3]
            pb, pt, _ = ptpool.next()
            kb.I('scalar', 'activation', [kb.pst[b]], [pt], pb[:, 0:w], kb.ps[b][:, 0:w], AF.Exp)
            for j in range(ndv):
                kb.I('tensor', 'matmul', [vtok, pt], [kb.pst[OB[j]]], kb.ps[OB[j]][:, 0:w], lhsT=Vs[:, kt, j * 128:(j + 1) * 128],
                     rhs=pb[:, 0:w], start=(kt == 0), stop=(kt == nkt - 1))
            kb.I('tensor', 'matmul', [obt, pt], [kb.pst[ZB]], kb.ps[ZB][:, 0:w], lhsT=ob, rhs=pb[:, 0:w],
                 start=(kt == 0), stop=(kt == nkt - 1))
        kb.I('vector', 'reciprocal', [kb.pst[ZB]], [rzt], rz[:, 0:w], kb.ps[ZB][:, 0:w])
        for j in range(ndv):
            kb.I('vector', 'tensor_tensor', [kb.pst[OB[j]], rzt], [], outs[j][:, c0:c1], kb.ps[OB[j]][:, 0:w], rz[:, 0:w], ALU.mult,
                 pw=[outtok[j]])


def build_l0b1():
    kb = KB()
    qT = kb.din("qT", [NQ, 128, T0], BF16)
    kT = kb.din("kT", [NKCH, 128, NKEY], BF16)
    Vd = kb.din("V", [NKEY, VW], BF16)
    lamv = kb.din("lamv", [128, 4])
    subln = kb.din("subln", [128, 2])
    cat = kb.dout("cat", [KC, 128, T0], BF16)
    cat_t = Tok('cat')
    x0a_t = Tok('x0a')
    of, oft, ob, obt = consts_common(kb)
    lam_s, lamt = load_vec(kb, lamv, 4)
    sub_s, subt = load_vec(kb, subln, 2)
    lp = kb.sb(2, F32)
    lpt = Tok('lp')
    kb.I('vector', 'tensor_tensor', [lamt], [], lp[:, 0:1], lam_s[:, 0:1], lam_s[:, 1:2], ALU.mult, pw=[lpt])
    kb.I('vector', 'tensor_tensor', [lamt], [], lp[:, 1:2], lam_s[:, 2:3], lam_s[:, 3:4], ALU.mult, pw=[lpt])
    kb.I('tensor', 'matmul', [lpt, oft], [kb.pst[7]], kb.ps[7][:, 0:2], lhsT=of, rhs=lp, start=True, stop=True)
    le = kb.sb(2, F32)
    let = Tok('le')
    kb.I('scalar', 'activation', [kb.pst[7]], [let], le, kb.ps[7][:, 0:2], AF.Exp)
    nlam = kb.sb(1, F32)
    nlt = Tok('nlam')
    kb.I('vector', 'tensor_tensor', [let], [nlt], nlam, le[:, 1:2], le[:, 0:1], ALU.subtract)
    kb.I('vector', 'tensor_scalar', [nlt], [nlt], nlam, nlam, -LAM_INIT0, 0.0, ALU.add, ALU.add)
    subg = kb.sb(2, F32)
    subgt = Tok('subg')
    kb.I('vector', 'tensor_scalar', [subt], [subgt], subg, sub_s, 1.0 - LAM_INIT0, 0.0, ALU.mult, ALU.add)
    m0 = kb.mark()
    kpool = Pool(kb, 'kp', 2, NKEY, BF16)
    vpool = Pool(kb, 'vp', 2, NKT * 256, BF16)
    qpool = Pool(kb, 'qp', 2, T0, BF16)
    ptpool = Pool(kb, 'pt', 3, 512, BF16)
    rz = kb.sb(512, F32)
    rzt = Tok('rz')
    o1 = [kb.sb(T0, F32) for _ in range(2)]
    o1t = [Tok('o1a'), Tok('o1b')]
    o2 = [kb.sb(T0, F32) for _ in range(2)]
    o2t = [Tok('o2a'), Tok('o2b')]
    sq = kb.sb(T0, F32)
    sqt = Tok('sq')
    rstd = kb.sb(T0, F32)
    rstdt = Tok('rstd')
    cpool = Pool(kb, 'cs', 2, T0, BF16)
    for g in range(4):
        kb_, kt_, kn_ = kpool.next()
        kb.dma('sync', kb_, kT[g], [], [kt_], "L" + kn_)
        vb_, vt_, vn_ = vpool.next()
        Vs = vb_[:, 0:NKT * 128].rearrange("p (t c) -> p t c", c=128)
        for hf in range(2):
            kb.dma('gpsimd', Vs[:, hf * 33:(hf + 1) * 33, :],
                   Vd[hf * 33 * 128:(hf + 1) * 33 * 128, g * 128:(g + 1) * 128].rearrange("(t p) c -> p t c", p=128), [], [], "L" + vn_, pw=[vt_])
        for hh in range(4):
            h = g * 4 + hh
            qb_, qt_, qn_ = qpool.next()
            kb.dma('sync', qb_, qT[h], [], [qt_], "L" + qn_)
            attn_unit(kb, qb_, qt_, kb_, kt_, Vs, vt_, 1, [o1[0]], [o1t[0]], ob, obt, ptpool, rz, rzt)
            cb_, ct_, cn_ = cpool.next()
            kb.I('scalar', 'activation', [o1t[0]], [ct_], cb_, o1[0], AF.Copy)
            kb.dma('sync', cat[h], cb_, [ct_], [], "S" + cn_, pw=[cat_t])
    for h in range(8):
        vb_, vt_, vn_ = vpool.next()
        Vs = vb_.rearrange("p (t c) -> p t c", c=256)
        for hf in range(2):
            kb.dma('gpsimd', Vs[:, hf * 33:(hf + 1) * 33, :],
                   Vd[hf * 33 * 128:(hf + 1) * 33 * 128, 512 + h * 256:512 + (h + 1) * 256].rearrange("(t p) c -> p t c", p=128), [], [], "L" + vn_, pw=[vt_])
        for m in range(2):
            kb_, kt_, kn_ = kpool.next()
            kb.dma('sync', kb_, kT[4 + h * 2 + m], [], [kt_], "L" + kn_)
            qb_, qt_, qn_ = qpool.next()
            kb.dma('sync', qb_, qT[16 + h * 2 + m], [], [qt_], "L" + qn_)
            oo, oot = (o1, o1t) if m == 0 else (o2, o2t)
            attn_unit(kb, qb_, qt_, kb_, kt_, Vs, vt_, 2, oo, oot, ob, obt, ptpool, rz, rzt)
        for j in range(2):
            kb.I('vector', 'scalar_tensor_tensor', [o1t[j], o2t[j], nlt], [o1t[j]], o1[j], o2[j], nlam[:, 0:1], o1[j], ALU.mult, ALU.add)
        for bi, (c0, c1) in enumerate(colblocks(T0)):
            b = 6 + bi % 2
            for j in range(2):
                kb.I('scalar', 'activation', [o1t[j]], [sqt], sq[:, c0:c1], o1[j][:, c0:c1], AF.Square)
                kb.I('tensor', 'matmul', [sqt, oft], [kb.pst[b]], kb.ps[b][:, 0:c1 - c0], lhsT=of, rhs=sq[:, c0:c1],
                     start=(j == 0), stop=(j == 1))
            kb.I('vector', 'tensor_scalar', [kb.pst[b]], [], rstd[:, c0:c1], kb.ps[b][:, 0:c1 - c0], 1.0 / 256, EPS, ALU.mult, ALU.add,
                 pw=[rstdt])
        kb.I('scalar', 'activation', [rstdt], [rstdt], rstd, rstd, AF.Sqrt)
        kb.I('vector', 'reciprocal', [rstdt], [rstdt], rstd, rstd)
        for j in range(2):
            cb_, ct_, cn_ = cpool.next()
            kb.I('vector', 'scalar_tensor_tensor', [o1t[j], rstdt, subgt], [ct_], cb_, o1[j], subg[:, j:j + 1], rstd, ALU.mult, ALU.mult)
            kb.dma('sync', cat[16 + h * 2 + j], cb_, [ct_], [], "S" + cn_, pw=[cat_t])
    return kb.finish()


def build_l0b2():
    kb = KB()
    cat = kb.din("cat", [KC, 128, T0], BF16)
    w_out = kb.din("w_out", [D, D])
    xT = kb.din("xT", [D, T0])
    modv = kb.din("modv", [128, 2 * 6 * KC])
    gn2 = kb.din("gn2", [128, KC])
    wg = kb.din("wg", [D, DFF])
    wu = kb.din("wu", [D, DFF])
    wd = kb.din("wd", [DFF, D])
    x1T = kb.dout("x1T", [D, T0])
    x0a = kb.dscr("x0a", [KC, 128, T0], F32)
    cat_t = Tok('cat')
    x0a_t = Tok('x0a')
    of, oft, ob, obt = consts_common(kb)
    mv_s, mvt = load_vec(kb, modv, 2 * 6 * KC)
    gn2_s, gn2t = load_vec(kb, gn2, KC)
    mv = mv_s.rearrange("p (v j k) -> p v j k", v=2, j=6)
    m0 = kb.mark()
    catT_flat = kb.sb(KC * T0, BF16)
    catT = catT_flat.rearrange("p (k t) -> p k t", k=KC)
    catT_t = Tok('catT')
    for k in range(KC):
        kb.dma('sync', catT[:, k, :], cat[k], [cat_t], [], f"Lcat{k % 2}", pw=[catT_t])
    wpool = Pool(kb, 'wp', 4, KC * 128, BF16)
    xpool = Pool(kb, 'xp', 2, T0, F32)
    spool = Pool(kb, 'st', 2, T0, F32)
    acc = kb.sb(T0, F32)
    acct = Tok('acc')
    tmp = kb.sb(T0, F32)
    tmpt = Tok('tmp')
    rstd2 = kb.sb(T0, F32)
    rstd2t = Tok('rstd2')
    blocks = colblocks(T0)
    seg_of = [0, 0, 1]

    def epi_out(n, tiles):
        xb, xt, xn = xpool.next()
        kb.dma('sync', xb, xT[n * 128:(n + 1) * 128, :], [], [xt], "L" + xn)
        sb_, st_, sn_ = spool.next()
        for bi, (ps, pt, c0, c1) in enumerate(tiles):
            v = seg_of[bi]
            kb.I('vector', 'scalar_tensor_tensor', [pt, xt, mvt], [], sb_[:, c0:c1], ps, mv[:, v, 2, n:n + 1], xb[:, c0:c1],
                 ALU.mult, ALU.add, pw=[st_])
        kb.dma('sync', x0a[n], sb_, [st_], [], "S" + sn_, pw=[x0a_t])
        if n == 0:
            kb.I('gpsimd', 'tensor_tensor', [st_], [acct], acc, sb_, sb_, ALU.mult)
        else:
            kb.I('scalar', 'activation', [st_], [tmpt], tmp, sb_, AF.Square)
            kb.I('gpsimd', 'tensor_tensor', [tmpt, acct], [acct], acc, acc, tmp, ALU.add)

    linear(kb, w_out, KC, list(range(KC)), lambda k, c0, c1: (catT[:, k, c0:c1], [catT_t]), blocks, epi_out, wpool, [0, 1, 2, 3, 4, 5])
    kb.P.barrier()
    rstd2p = rstd2
    sumsq_finish(kb, acc, acct, T0, of, oft, 1.0 / D, rstd2p, rstd2t, [6, 7])
    A2 = [kb.sb(KC, F32) for _ in range(2)]
    A2t = Tok('A2')
    for v in range(2):
        kb.I('vector', 'scalar_tensor_tensor', [mvt, gn2t], [], A2[v], mv[:, v, 4, :], 1.0, gn2_s, ALU.add, ALU.mult, pw=[A2t])
    kb.P.barrier()
    kb.off = m0
    rstd2k = kb.sb(T0, F32)
    A2k = [kb.sb(KC, F32) for _ in range(2)]
    keept = Tok('keep')
    kb.I('vector', 'tensor_copy', [rstd2t], [], rstd2k, rstd2p, pw=[keept])
    for v in range(2):
        kb.I('vector', 'tensor_copy', [A2t], [], A2k[v], A2[v], pw=[keept])
    kb.P.barrier()
    m1 = kb.mark()
    for ti, (c0, c1) in enumerate([(0, 512), (512, 1024), (1024, 1280)]):
        v = seg_of[ti]
        Tt = c1 - c0
        h2_flat = kb.sb(KC * Tt, BF16)
        h2 = h2_flat.rearrange("p (k t) -> p k t", k=KC)
        h2t = Tok('h2')
        aT_flat = kb.sb(NF * Tt, BF16)
        aT = aT_flat.rearrange("p (f t) -> p f t", f=NF)
        aTt = Tok('aT')
        wpool = Pool(kb, 'wp', 6, KC * 128, BF16)
        xpool = Pool(kb, 'xp', 3, Tt, F32)
        tpool = Pool(kb, 'tp', 2, Tt, F32)
        spool = Pool(kb, 'st', 2, Tt, F32)
        for k in range(KC):
            xb, xt, xn = xpool.next()
            kb.dma('sync', xb, x0a[k][:, c0:c1], [x0a_t], [xt], "L" + xn)
            tb, tt_, _ = tpool.next()
            kb.I('vector', 'scalar_tensor_tensor', [xt, keept], [tt_], tb, xb, A2k[v][:, k:k + 1], rstd2k[:, c0:c1], ALU.mult, ALU.mult)
            kb.I('scalar', 'activation', [tt_, mvt], [], h2[:, k, :], tb, AF.Identity, bias=mv[:, v, 3, k:k + 1], scale=1.0, pw=[h2t])
        for f in range(NF):
            pair = (f % 3) * 2
            for (W, b) in ((wg, pair), (wu, pair + 1)):
                wb, wt, wn = wpool.next()
                wv = wb.rearrange("p (k n) -> p k n", n=128)
                kb.dma('gpsimd', wv, W[:, f * 128:(f + 1) * 128].rearrange("(k p) n -> p k n", p=128), [], [wt], "L" + wn)
                for k in range(KC):
                    kb.I('tensor', 'matmul', [wt, h2t], [kb.pst[b]], kb.ps[b][:, 0:Tt], lhsT=wv[:, k, :], rhs=h2[:, k, :],
                         start=(k == 0), stop=(k == KC - 1))
            tb, tt_, _ = tpool.next()
            kb.I('scalar', 'activation', [kb.pst[pair]], [tt_], tb, kb.ps[pair][:, 0:Tt], AF.Silu)
            kb.I('vector', 'tensor_tensor', [tt_, kb.pst[pair + 1]], [], aT[:, f, :], tb, kb.ps[pair + 1][:, 0:Tt], ALU.mult, pw=[aTt])

        def epi_dn(n, tiles, c0=c0, c1=c1, v=v, xpool=xpool, spool=spool):
            ps, pt, _, _ = tiles[0]
            xb, xt, xn = xpool.next()
            kb.dma('sync', xb, x0a[n][:, c0:c1], [x0a_t], [xt], "L" + xn)
            sb_, st_, sn_ = spool.next()
            kb.I('vector', 'scalar_tensor_tensor', [pt, xt, mvt], [st_], sb_, ps, mv[:, v, 5, n:n + 1], xb, ALU.mult, ALU.add)
            kb.dma('sync', x1T[n * 128:(n + 1) * 128, c0:c1], sb_, [st_], [], "S" + sn_)

        linear(kb, wd, NF, list(range(KC)), lambda k, a, b_, aT=aT, aTt=aTt: (aT[:, k, a:b_], [aTt]), [(0, Tt)], epi_dn, wpool,
               [0, 1, 2, 3, 4, 5])
        kb.release(m1)
    return kb.finish()


def host_l0b1(r0a, lq1, lk1, lq2, lk2, subln):
    kT_all = np.ascontiguousarray(np.concatenate([r0a[0]["kT"][:, :, TL:T0]] + [r0a[i]["kT"][:, :, 0:TL] for i in range(NCORES)], axis=2))
    V_all = np.ascontiguousarray(np.concatenate([r0a[0]["V"][TL:T0]] + [r0a[i]["V"][0:TL] for i in range(NCORES)], axis=0))
    lamv = np.ascontiguousarray(np.stack([lq1, lk1, lq2, lk2], axis=1).astype(np.float32))
    sub = np.ascontiguousarray(subln.reshape(2, 128).T.astype(np.float32))
    common = dict(host_consts(), kT=kT_all, V=V_all, lamv=lamv, subln=sub)
    in_maps = []
    for i in range(NCORES):
        m = dict(common)
        m["qT"] = np.ascontiguousarray(r0a[i]["qT"])
        in_maps.append(m)
    return run("l0b1", build_l0b1, in_maps)


def host_l0b2(r0b1, x, ctx, mod0, norm_ffn, w_out, wg, wu, wd):
    xTf = np.ascontiguousarray(x.T)
    cT = np.ascontiguousarray(ctx.T)
    common = dict(host_consts(), w_out=w_out, modv=modv_layout(mod0), gn2=fm(norm_ffn), wg=wg, wu=wu, wd=wd)
    in_maps = []
    for i in range(NCORES):
        m = dict(common)
        m["cat"] = np.ascontiguousarray(r0b1[i]["cat"])
        m["xT"] = np.ascontiguousarray(np.concatenate([xTf[:, i * TL:(i + 1) * TL], cT], axis=1))
        in_maps.append(m)
    return run("l0b2", build_l0b2, in_maps)


TE = TL + 3
T1 = TE + TC
XRW = TE + TC + 3


def build_l1a():
    kb = KB()
    xT = kb.din("xT", [D, T1])
    modv = kb.din("modv", [128, 2 * 6 * KC])
    gn = kb.din("gn", [128, KC])
    w_in = kb.din("w_in", [D, 2 * D])
    hmask = kb.din("hmask", [128, TE])
    convw = kb.din("convw", [128, 4 * KC])
    convb = kb.din("convb", [128, KC])
    gaw = kb.din("gaw", [2, 16, 256, 256])
    gxw = kb.din("gxw", [2, 16, 256, 256])
    gab = kb.din("gab", [128, 2 * KC])
    gxb = kb.din("gxb", [128, 2 * KC])
    lam = kb.din("lam", [128, 2 * KC])
    G = kb.dout("G", [KC, 128, TL], BF16)
    S0 = kb.dout("S0", [KC, 128, TL])
    Pf = kb.dout("Pf", [KC, 128, TL])
    Pb = kb.dout("Pb", [KC, 128, TL])
    AE = kb.dout("AE", [128, KC * 6])
    xc = kb.dscr("xc", [KC, 128, TL + TC])
    xc_t = Tok('xc')
    of, oft, ob, obt = consts_common(kb)
    mv_s, mvt = load_vec(kb, modv, 2 * 6 * KC)
    gn_s, gnt = load_vec(kb, gn, KC)
    hm_s, hmt = load_vec(kb, hmask, TE)
    cw_s, cwt = load_vec(kb, convw, 4 * KC)
    cb_s, cbt = load_vec(kb, convb, KC)
    gab_s, gabt = load_vec(kb, gab, 2 * KC)
    gxb_s, gxbt = load_vec(kb, gxb, 2 * KC)
    lam_s, lamt = load_vec(kb, lam, 2 * KC)
    cw = cw_s.rearrange("p (j k) -> p j k", j=4)
    mv = mv_s.rearrange("p (v j k) -> p v j k", v=2, j=6)
    A = [kb.sb(KC, F32) for _ in range(2)]
    At = Tok('A')
    for v in range(2):
        kb.I('vector', 'scalar_tensor_tensor', [mvt, gnt], [], A[v], mv[:, v, 1, :], 1.0, gn_s, ALU.add, ALU.mult, pw=[At])
    Bv = [mv[:, v, 0, :] for v in range(2)]
    cvec = kb.sb(2 * KC, F32)
    cvt = Tok('cvec')
    kb.I('scalar', 'activation', [lamt], [cvt], cvec, lam_s, AF.Exp, scale=-1.0)
    kb.I('scalar', 'activation', [cvt], [cvt], cvec, cvec, AF.Ln, bias=1.0, scale=1.0)
    kb.I('vector', 'tensor_scalar', [cvt], [cvt], cvec, cvec, -8.0, 0.0, ALU.mult, ALU.add)
    aeb = kb.sb(KC * 6, F32)
    aet = Tok('ae')
    aev = aeb.rearrange("p (k j) -> p k j", j=6)
    zeros = kb.sb(TL, F32)
    zt = Tok('zeros')
    kb.I('vector', 'memset', [], [zt], zeros, 0.0)
    hT_flat = kb.sb(KC * T1, BF16)
    hT = hT_flat.rearrange("p (k t) -> p k t", k=KC)
    ht = Tok('hT')
    m0 = kb.mark()
    xpool = Pool(kb, 'xp', 2, T1, F32)
    modulate(kb, lambda k: xT[k * 128:(k + 1) * 128, :], T1, A, Bv, [At, mvt], hT, ht, of, oft, [6, 7], xpool,
             segs=[(0, TE, 0), (TE, T1, 1)])
    kb.release(m0)
    wpool = Pool(kb, 'wp', 4, KC * 128, BF16)
    gpool = Pool(kb, 'gs', 2, TL, BF16)

    def epi_y(n, tiles):
        gb, gt, gn_ = gpool.next()
        for (ps, pt, c0, c1) in tiles:
            kb.I('scalar', 'activation', [pt], [], gb[:, c0:c1], ps, AF.Gelu_apprx_tanh, pw=[gt])
        kb.dma('sync', G[n], gb, [gt], [], "S" + gn_)

    linear(kb, w_in, KC, list(range(KC)), lambda k, c0, c1: (hT[:, k, 2 + c0:2 + c1], [ht]), colblocks(TL), epi_y, wpool,
           [0, 1, 2, 3, 4, 5])
    xrpool = Pool(kb, 'xr', 2, XRW, F32)
    for i in range(2):
        kb.I('vector', 'memset', [], [], xrpool.bufs[i][:, TE:TE + 2], 0.0, pw=[xrpool.toks[i]])
        kb.I('vector', 'memset', [], [], xrpool.bufs[i][:, XRW - 1:XRW], 0.0, pw=[xrpool.toks[i]])
    xcpool = Pool(kb, 'xc', 2, TL + TC, F32)

    def epi_xr(n, tiles):
        k = n - KC
        xb, xt, _ = xrpool.next()
        for bi, (ps, pt, c0, c1) in enumerate(tiles):
            if bi < 2:
                kb.I('vector', 'tensor_tensor', [pt, hmt], [], xb[:, c0:c1], ps, hm_s[:, c0:c1], ALU.mult, pw=[xt])
            else:
                kb.I('vector', 'tensor_tensor', [pt, hmt], [], xb[:, 1024:TE], ps[:, 0:3], hm_s[:, 1024:TE], ALU.mult, pw=[xt])
                kb.I('scalar', 'activation', [pt], [], xb[:, TE + 2:TE + 2 + TC], ps[:, 3:3 + TC], AF.Copy, pw=[xt])
        cbuf, ct, cn = xcpool.next()
        for (o0, o1, i0) in [(0, TL, 0), (TL, TL + TC, TE)]:
            w = o1 - o0
            kb.I('scalar', 'activation', [xt, cwt, cbt], [], cbuf[:, o0:o1], xb[:, i0:i0 + w], AF.Identity,
                 bias=cb_s[:, k:k + 1], scale=cw[:, 0, k:k + 1], pw=[ct])
            for j in range(1, 4):
                kb.I('vector', 'scalar_tensor_tensor', [xt, cwt, ct], [ct], cbuf[:, o0:o1], xb[:, i0 + j:i0 + j + w], cw[:, j, k:k + 1],
                     cbuf[:, o0:o1], ALU.mult, ALU.add)
        kb.dma('sync', xc[k], cbuf, [ct], [], "S" + cn, pw=[xc_t])

    linear(kb, w_in, KC, list(range(KC, 2 * KC)), lambda k, c0, c1: (hT[:, k, c0:c1], [ht]), colblocks(T1), epi_xr, wpool,
           [0, 1, 2, 3, 4, 5])
    kb.off = m0 - KC * T1
    kb.P.barrier()
    TT = TL + TC
    xcl = Pool(kb, 'xl', 4, TT, F32)
    xcb = Pool(kb, 'xb', 4, TT, BF16)
    gwp = Pool(kb, 'gw', 4, 2 * 256, BF16)
    rbuf = kb.sb(TT, F32)
    rt = Tok('r')
    ibuf = kb.sb(TT, F32)
    it_ = Tok('i')
    abuf = kb.sb(TT, F32)
    at_ = Tok('a')
    a2buf = kb.sb(TT, F32)
    a2t = Tok('a2')
    bbuf = kb.sb(TT, F32)
    bt_ = Tok('b')
    hcb = kb.sb(TC, F32)
    hct = Tok('hc')
    hlb = kb.sb(TL, F32)
    hlt = Tok('hl')
    s0p = Pool(kb, 's0', 2, TL, F32)
    pfp = Pool(kb, 'pf', 2, TL, F32)
    pbp = Pool(kb, 'pb', 2, TL, F32)
    blocks = colblocks(TT)
    for blk in range(16):
        xl = []
        xbf = []
        for kk in range(2):
            lb, lt, ln_ = xcl.next()
            kb.dma('sync', lb, xc[2 * blk + kk], [xc_t], [lt], "L" + ln_)
            bb_, btk, _ = xcb.next()
            kb.I('scalar', 'activation', [lt], [btk], bb_, lb, AF.Copy)
            xl.append((lb, lt))
            xbf.append((bb_, btk))
        gws = {}
        for d in range(2):
            for gi, Wsrc in enumerate((gaw, gxw)):
                wb, wt, wn = gwp.next()
                wv = wb.rearrange("p (k n) -> p k n", n=256)
                kb.dma('gpsimd', wv, Wsrc[d, blk].rearrange("(k p) n -> p k n", p=128), [], [wt], "L" + wn)
                gws[(d, gi)] = (wv, wt)
        for e in range(2):
            c = 2 * blk + e
            s0b, s0t, s0n = s0p.next()
            pfb, pft, pfn = pfp.next()
            pbb, pbt, pbn = pbp.next()
            for d in range(2):
                for gi, (dst, dtok, bias_s, btok) in enumerate(((rbuf, rt, gab_s, gabt), (ibuf, it_, gxb_s, gxbt))):
                    wv, wt = gws[(d, gi)]
                    for bi, (c0, c1) in enumerate(blocks):
                        b = gi * 3 + bi
                        for kk in range(2):
                            kb.I('tensor', 'matmul', [wt, xbf[kk][1]], [kb.pst[b]], kb.ps[b][:, 0:c1 - c0],
                                 lhsT=wv[:, kk, e * 128:(e + 1) * 128], rhs=xbf[kk][0][:, c0:c1], start=(kk == 0), stop=(kk == 1))
                        kb.I('scalar', 'activation', [kb.pst[b], btok], [], dst[:, c0:c1], kb.ps[b][:, 0:c1 - c0], AF.Sigmoid,
                             bias=bias_s[:, d * KC + c:d * KC + c + 1], scale=1.0, pw=[dtok])
                kb.I('scalar', 'activation', [rt, cvt], [at_], abuf, rbuf, AF.Exp, scale=cvec[:, d * KC + c:d * KC + c + 1])
                kb.I('vector', 'tensor_tensor', [at_], [a2t], a2buf, abuf, abuf, ALU.mult)
                kb.I('scalar', 'activation', [a2t], [a2t], a2buf, a2buf, AF.Sqrt, bias=1.0, scale=-1.0)
                kb.I('vector', 'tensor_tensor', [it_, xl[e][1]], [bt_], bbuf, ibuf, xl[e][0], ALU.mult)
                kb.I('vector', 'tensor_tensor', [bt_, a2t], [bt_], bbuf, bbuf, a2buf, ALU.mult)
                rv = (lambda ap: ap) if d == 0 else (lambda ap: ap[:, ::-1])
                kb.I('vector', 'tensor_tensor_scan', [at_, bt_], [hct], rv(hcb), rv(abuf[:, TL:TT]), rv(bbuf[:, TL:TT]), 0.0, ALU.mult, ALU.add)
                hdst, hdt = (s0b, s0t) if d == 0 else (hlb, hlt)
                pdst, pdt = (pfb, pft) if d == 0 else (pbb, pbt)
                kb.I('vector', 'tensor_tensor_scan', [at_, bt_], [hdt], rv(hdst), rv(abuf[:, 0:TL]), rv(bbuf[:, 0:TL]), 0.0, ALU.mult, ALU.add)
                kb.I('vector', 'tensor_tensor_scan', [at_, zt], [pdt], rv(pdst), rv(abuf[:, 0:TL]), rv(zeros), 1.0, ALU.mult, ALU.add)
                endc = TL - 1 if d == 0 else 0
                kb.I('vector', 'tensor_copy', [pdt], [], aev[:, c, 2 * d:2 * d + 1], pdst[:, endc:endc + 1], pw=[aet])
                kb.I('vector', 'tensor_copy', [hdt], [], aev[:, c, 2 * d + 1:2 * d + 2], hdst[:, endc:endc + 1], pw=[aet])
                ce = TC - 1 if d == 0 else 0
                kb.I('vector', 'tensor_copy', [hct], [], aev[:, c, 4 + d:5 + d], hcb[:, ce:ce + 1], pw=[aet])
                if d == 1:
                    kb.I('vector', 'tensor_tensor', [s0t, hlt], [s0t], s0b, s0b, hlb, ALU.add)
            kb.dma('sync', S0[c], s0b, [s0t], [], "S" + s0n)
            kb.dma('sync', Pf[c], pfb, [pft], [], "S" + pfn)
            kb.dma('sync', Pb[c], pbb, [pbt], [], "S" + pbn)
    kb.dma('sync', AE, aeb, [aet], [], "Sae")
    return kb.finish()


def host_l1a(x1_lat, ctx1, mod1, norm_mix, w_in, conv_w, conv_b, ga_w, ga_b, gx_w, gx_b, lru_lam):
    xTf = np.ascontiguousarray(x1_lat.T)
    cT = np.ascontiguousarray(ctx1.T)
    xpad = np.concatenate([np.zeros((D, 2), np.float32), xTf, np.zeros((D, 1), np.float32)], axis=1)
    common = dict(host_consts(), modv=modv_layout(mod1), gn=fm(norm_mix), w_in=w_in,
                  convw=np.ascontiguousarray(fm(conv_w).reshape(128, 4 * KC)), convb=fm(conv_b),
                  gaw=ga_w, gxw=gx_w, gab=np.ascontiguousarray(fm(ga_b).reshape(128, 2 * KC)),
                  gxb=np.ascontiguousarray(fm(gx_b).reshape(128, 2 * KC)), lam=np.ascontiguousarray(fm(lru_lam).reshape(128, 2 * KC)))
    in_maps = []
    for i in range(NCORES):
        m = dict(common)
        m["xT"] = np.ascontiguousarray(np.concatenate([xpad[:, i * TL:i * TL + TE], cT], axis=1))
        hm = np.ones((128, TE), np.float32)
        if i == 0:
            hm[:, 0:2] = 0.0
        if i == NCORES - 1:
            hm[:, TE - 1:TE] = 0.0
        m["hmask"] = hm
        in_maps.append(m)
    return run("l1a", build_l1a, in_maps)


NE = 8
FH = 16


def build_l1b():
    kb = KB()
    S0 = kb.din("S0", [KC, 128, TL])
    Pf = kb.din("Pf", [KC, 128, TL])
    Pb = kb.din("Pb", [KC, 128, TL])
    G = kb.din("G", [KC, 128, TL], BF16)
    AEa = kb.din("AEa", [128, NCORES * KC * 6])
    sel = kb.din("sel", [128, NCORES])
    x1T = kb.din("x1T", [D, TL])
    modv = kb.din("modv", [128, 2 * 6 * KC])
    gn2 = kb.din("gn2", [128, KC])
    w_out = kb.din("w_out", [D, D])
    rw = kb.din("rw", [128, KC * NE])
    rb = kb.din("rb", [NE, 1])
    ident = kb.din("ident", [128, 128])
    outT = kb.dout("outT", [D, TL])
    h3o = kb.dout("h3T", [KC, 128, TL], BF16)
    comb_o = kb.dout("comb", [128, (TL // 128) * NE])
    mask_o = kb.dout("mask", [128, (TL // 128) * NE])
    out_t = [Tok(f'out{n}') for n in range(KC)]
    of, oft, ob, obt = consts_common(kb)
    mv_s, mvt = load_vec(kb, modv, 2 * 6 * KC)
    gn2_s, gn2t = load_vec(kb, gn2, KC)
    ae_s, aet = load_vec(kb, AEa, NCORES * KC * 6)
    sel_s, selt = load_vec(kb, sel, NCORES)
    rw_s, rwt = load_vec(kb, rw, KC * NE)
    id_s, idt = load_vec(kb, ident, 128)
    rb_s = kb.sb(1, F32)
    rbt = Tok('rb')
    kb.dma('sync', rb_s[0:NE, :], rb, [], [rbt], "c0")
    mv = mv_s.rearrange("p (v j k) -> p v j k", v=2, j=6)
    ae = ae_s.rearrange("p (r k j) -> p r k j", r=NCORES, j=6)
    rwv = rw_s.rearrange("p (k e) -> p k e", e=NE)
    hcur = kb.sb(KC, F32)
    hct = Tok('hcur')
    hin = [kb.sb(KC, F32) for _ in range(2)]
    hint = [Tok('hinf'), Tok('hinb')]
    for d in range(2):
        order = list(range(NCORES)) if d == 0 else list(range(NCORES - 1, -1, -1))
        kb.I('vector', 'tensor_copy', [aet], [hct], hcur, ae[:, 0, :, 4 + d])
        for ci, r in enumerate(order):
            if ci == 0:
                kb.I('vector', 'tensor_scalar', [hct, selt], [hint[d]], hin[d], hcur, sel_s[:, r:r + 1], 0.0, ALU.mult, ALU.add)
            else:
                kb.I('vector', 'scalar_tensor_tensor', [hct, selt, hint[d]], [hint[d]], hin[d], hcur, sel_s[:, r:r + 1], hin[d],
                     ALU.mult, ALU.add)
            if ci < NCORES - 1:
                kb.I('vector', 'tensor_tensor', [hct, aet], [hct], hcur, hcur, ae[:, r, :, 2 * d], ALU.mult)
                kb.I('vector', 'tensor_tensor', [hct, aet], [hct], hcur, hcur, ae[:, r, :, 2 * d + 1], ALU.add)
    m0 = kb.mark()
    mT_flat = kb.sb(KC * TL, BF16)
    mT = mT_flat.rearrange("p (k t) -> p k t", k=KC)
    mTt = Tok('mT')
    m1 = kb.mark()
    lp = [Pool(kb, nm, 2, TL, F32) for nm in ('fa', 'fb', 'fc')]
    gp = Pool(kb, 'fg', 2, TL, BF16)
    for k in range(KC):
        bufs = []
        for pl, src in zip(lp, (S0, Pf, Pb)):
            b_, t_, n_ = pl.next()
            kb.dma('sync', b_, src[k], [], [t_], "L" + n_)
            bufs.append((b_, t_))
        gb, gt, gn_ = gp.next()
        kb.dma('sync', gb, G[k], [], [gt], "L" + gn_)
        (sb_, st_), (fb_, ft_), (bb_, bt_) = bufs
        kb.I('vector', 'scalar_tensor_tensor', [ft_, st_, hint[0]], [st_], sb_, fb_, hin[0][:, k:k + 1], sb_, ALU.mult, ALU.add)
        kb.I('vector', 'scalar_tensor_tensor', [bt_, st_, hint[1]], [st_], sb_, bb_, hin[1][:, k:k + 1], sb_, ALU.mult, ALU.add)
        kb.I('vector', 'tensor_tensor', [st_, gt], [], mT[:, k, :], sb_, gb, ALU.mult, pw=[mTt])
    kb.release(m1)
    wpool = Pool(kb, 'wp', 4, KC * 128, BF16)
    xpool = Pool(kb, 'xp', 2, TL, F32)
    spool = Pool(kb, 'st', 2, TL, F32)
    acc = kb.sb(TL, F32)
    acct = Tok('acc')
    tmp = kb.sb(TL, F32)
    tmpt = Tok('tmp')
    blocks = colblocks(TL)

    def epi_out(n, tiles):
        xb, xt, xn = xpool.next()
        kb.dma('sync', xb, x1T[n * 128:(n + 1) * 128, :], [], [xt], "L" + xn)
        sb_, st_, sn_ = spool.next()
        for (ps, pt, c0, c1) in tiles:
            kb.I('vector', 'scalar_tensor_tensor', [pt, xt, mvt], [], sb_[:, c0:c1], ps, mv[:, 0, 2, n:n + 1], xb[:, c0:c1],
                 ALU.mult, ALU.add, pw=[st_])
        kb.dma('sync', outT[n * 128:(n + 1) * 128, :], sb_, [st_], [out_t[n]], "S" + sn_)
        if n == 0:
            kb.I('gpsimd', 'tensor_tensor', [st_], [acct], acc, sb_, sb_, ALU.mult)
        else:
            kb.I('scalar', 'activation', [st_], [tmpt], tmp, sb_, AF.Square)
            kb.I('gpsimd', 'tensor_tensor', [tmpt, acct], [acct], acc, acc, tmp, ALU.add)

    linear(kb, w_out, KC, list(range(KC)), lambda k, c0, c1: (mT[:, k, c0:c1], [mTt]), blocks, epi_out, wpool, [0, 1, 2, 3, 4, 5])
    kb.P.barrier()
    kb.off = m0
    rstd3 = kb.sb(TL, F32)
    rstd3t = Tok('rstd3')
    sumsq_finish(kb, acc, acct, TL, of, oft, 1.0 / D, rstd3, rstd3t, [6, 7])
    A3 = kb.sb(KC, F32)
    A3t = Tok('A3')
    kb.I('vector', 'scalar_tensor_tensor', [mvt, gn2t], [A3t], A3, mv[:, 0, 4, :], 1.0, gn2_s, ALU.add, ALU.mult)
    kb.P.barrier()
    h3_flat = kb.sb(KC * TL, BF16)
    h3 = h3_flat.rearrange("p (k t) -> p k t", k=KC)
    h3t = Tok('h3')
    m2 = kb.mark()
    xpool = Pool(kb, 'xp', 2, TL, F32)
    tpool = Pool(kb, 'tp', 2, TL, F32)
    hfp = Pool(kb, 'hf', 2, TL, F32)
    for k in range(KC):
        xb, xt, xn = xpool.next()
        kb.dma('sync', xb, outT[k * 128:(k + 1) * 128, :], [out_t[k]], [xt], "L" + xn)
        tb, tt_, _ = tpool.next()
        kb.I('vector', 'scalar_tensor_tensor', [xt, rstd3t, A3t], [tt_], tb, xb, A3[:, k:k + 1], rstd3, ALU.mult, ALU.mult)
        hb, hbt, _ = hfp.next()
        kb.I('scalar', 'activation', [tt_, mvt], [hbt], hb, tb, AF.Identity, bias=mv[:, 0, 3, k:k + 1], scale=1.0)
        kb.I('gpsimd', 'tensor_copy', [hbt], [], h3[:, k, :], hb, pw=[h3t])
        for bi, (c0, c1) in enumerate(blocks):
            kb.I('tensor', 'matmul', [hbt, rwt], [kb.pst[6 + bi]], kb.ps[6 + bi][0:NE, 0:512], lhsT=rwv[:, k, :], rhs=hb[:, c0:c1],
                 start=(k == 0), stop=(k == KC - 1))
    lg = kb.sb(TL, F32)
    lgt = Tok('lg')
    for bi, (c0, c1) in enumerate(blocks):
        kb.I('scalar', 'activation', [kb.pst[6 + bi], rbt], [], lg[0:NE, c0:c1], kb.ps[6 + bi][0:NE, 0:512], AF.Identity,
             bias=rb_s[0:NE, 0:1], scale=1.0, pw=[lgt])
    NT = TL // 128
    for t in range(NT):
        kb.I('tensor', 'matmul', [lgt, idt], [], kb.ps[0][:, t * NE:(t + 1) * NE], lhsT=lg[0:NE, t * 128:(t + 1) * 128],
             rhs=id_s[0:NE, 0:NE], start=True, stop=True, pw=[kb.pst[0]])
    L = kb.sb(NT * NE, F32)
    Lt = Tok('L')
    kb.I('vector', 'tensor_copy', [kb.pst[0]], [Lt], L, kb.ps[0][:, 0:NT * NE])
    L3 = L.rearrange("p (t e) -> p t e", e=NE)
    mx1 = kb.sb(NT, F32)
    mx2 = kb.sb(NT, F32)
    w1 = kb.sb(NT * NE, F32)
    w13 = w1.rearrange("p (t e) -> p t e", e=NE)
    w2 = kb.sb(NT * NE, F32)
    w23 = w2.rearrange("p (t e) -> p t e", e=NE)
    tk = Tok('topk')

    def bc(v):
        return v.unsqueeze(2).to_broadcast([128, NT, NE])
    kb.I('vector', 'tensor_reduce', [Lt], [tk], mx1, L3, AX.X, ALU.max)
    kb.I('vector', 'tensor_tensor', [Lt, tk], [tk], w13, L3, bc(mx1), ALU.is_equal)
    kb.I('vector', 'scalar_tensor_tensor', [Lt, tk], [tk], w13, w13, -1e30, L3, ALU.mult, ALU.add)
    kb.I('vector', 'tensor_reduce', [tk], [tk], mx2, w13, AX.X, ALU.max)
    kb.I('vector', 'tensor_tensor', [Lt, tk], [tk], w13, L3, bc(mx2), ALU.is_ge)
    msk = kb.sb(NT * NE, F32)
    kb.I('vector', 'tensor_copy', [tk], [tk], msk, w1)
    kb.I('vector', 'tensor_tensor', [Lt, tk], [tk], w23, L3, bc(mx1), ALU.subtract)
    kb.I('scalar', 'activation', [tk], [tk], w2, w2, AF.Exp)
    kb.I('vector', 'tensor_tensor', [tk], [tk], w1, w1, w2, ALU.mult)
    kb.I('vector', 'tensor_reduce', [tk], [tk], mx1, w13, AX.X, ALU.add)
    kb.I('vector', 'reciprocal', [tk], [tk], mx1, mx1)
    kb.I('vector', 'tensor_tensor', [tk], [tk], w13, w13, bc(mx1), ALU.mult)
    kb.dma('sync', comb_o, w1, [tk], [], "Scomb")
    kb.dma('sync', mask_o, msk, [tk], [], "Smask")
    for k in range(KC):
        pass
    kb.dma('sync', h3o.rearrange("k p t -> p k t"), h3, [h3t], [], "Sh3")
    return kb.finish()


def host_l1b(r1a, x1_lat, mod1, norm_ffn, w_out, router_w, router_b):
    AEa = np.ascontiguousarray(np.concatenate([r1a[i]["AE"].reshape(128, 1, KC * 6) for i in range(NCORES)], axis=1).reshape(128, -1))
    xTf = np.ascontiguousarray(x1_lat.T)
    rwf = np.ascontiguousarray(np.moveaxis(router_w.reshape(KC, 128, NE), 1, 0).reshape(128, KC * NE))
    common = dict(host_consts(), AEa=AEa, modv=modv_layout(mod1), gn2=fm(norm_ffn), w_out=w_out, rw=rwf,
                  rb=np.ascontiguousarray(router_b.reshape(NE, 1)), ident=np.eye(128, dtype=np.float32))
    in_maps = []
    for i in range(NCORES):
        m = dict(common)
        for nm in ("S0", "Pf", "Pb", "G"):
            m[nm] = np.ascontiguousarray(r1a[i][nm])
        s = np.zeros((128, NCORES), np.float32)
        s[:, i] = 1.0
        m["sel"] = s
        m["x1T"] = np.ascontiguousarray(xTf[:, i * TL:(i + 1) * TL])
        in_maps.append(m)
    return run("l1b", build_l1b, in_maps)


def build_l1c(NS, Cp):
    kb = KB()
    g2 = kb.din("g2", [128, KC])
    slots = []
    for j in range(NS):
        slots.append((kb.din(f"hg{j}", [KC, 128, Cp], BF16), kb.din(f"wrow{j}", [128, Cp]), kb.din(f"ewg{j}", [D, D]),
                      kb.din(f"ewu{j}", [D, D]), kb.din(f"ewd{j}", [D, D]), kb.dout(f"yT{j}", [KC, 128, Cp])))
    g2_s, g2t = load_vec(kb, g2, KC)
    m0 = kb.mark()
    for (hg, wrow, ewg, ewu, ewd, yT) in slots:
      for (t0, t1) in colblocks(Cp, 1024):
          Tt = t1 - t0
          blocks = colblocks(Tt)
          nb = len(blocks)
          hT_flat = kb.sb(KC * Tt, BF16)
          hT = hT_flat.rearrange("p (k t) -> p k t", k=KC)
          ht = Tok('hT')
          for k in range(KC):
              kb.dma('sync', hT[:, k, :], hg[k][:, t0:t1], [], [], f"Lh{k % 2}", pw=[ht])
          wb = kb.sb(Tt, F32)
          wbt = Tok('wb')
          kb.dma('sync', wb, wrow[:, t0:t1], [], [wbt], "Lwb")
          aT_flat = kb.sb(KC * Tt, BF16)
          aT = aT_flat.rearrange("p (f t) -> p f t", f=KC)
          aTt = Tok('aT')
          wpool = Pool(kb, 'wp', 4, KC * 128, BF16)
          sgp = Pool(kb, 'sg', 2, Tt, BF16)
          tgp = Pool(kb, 'tg', 2, Tt, BF16)
          spool = Pool(kb, 'so', 2, Tt, F32)
          pit = 0
          for f in range(KC):
              bs = (pit % 2) * 4
              pit += 1
              for wi, W in enumerate((ewg, ewu)):
                  wbuf, wt, wn = wpool.next()
                  wv = wbuf.rearrange("p (k n) -> p k n", n=128)
                  kb.dma('gpsimd', wv, W[:, f * 128:(f + 1) * 128].rearrange("(k p) n -> p k n", p=128), [], [wt], "L" + wn)
                  for k in range(KC):
                      for bi, (c0, c1) in enumerate(blocks):
                          b = bs + wi * 2 + bi
                          kb.I('tensor', 'matmul', [wt, ht], [kb.pst[b]], kb.ps[b][:, 0:c1 - c0], lhsT=wv[:, k, :], rhs=hT[:, k, c0:c1],
                               start=(k == 0), stop=(k == KC - 1))
              sg, sgt, _ = sgp.next()
              tg, tgt, _ = tgp.next()
              for bi, (c0, c1) in enumerate(blocks):
                  kb.I('scalar', 'activation', [kb.pst[bs + bi]], [], sg[:, c0:c1], kb.ps[bs + bi][:, 0:c1 - c0], AF.Silu, pw=[sgt])
              for bi, (c0, c1) in enumerate(blocks):
                  kb.I('vector', 'tensor_tensor', [sgt, kb.pst[bs + 2 + bi]], [], tg[:, c0:c1], sg[:, c0:c1], kb.ps[bs + 2 + bi][:, 0:c1 - c0],
                       ALU.mult, pw=[tgt])
              kb.I('gpsimd', 'tensor_tensor', [tgt, wbt], [], aT[:, f, :], tg, wb, ALU.mult, pw=[aTt])

          def epi_dn(n, tiles, spool=spool, t0=t0, t1=t1, yT=yT):
              sb_, st_, sn_ = spool.next()
              for (ps, pt, c0, c1) in tiles:
                  kb.I('scalar', 'activation', [pt, g2t], [], sb_[:, c0:c1], ps, AF.Identity, scale=g2_s[:, n:n + 1], bias=0.0, pw=[st_])
              kb.dma('sync', yT[n][:, t0:t1], sb_, [st_], [], "S" + sn_)

          banks = [0, 1, 2, 3, 4, 5, 6, 7] if nb == 2 else [0, 1, 2, 3, 4, 5, 6, 7][:8 // nb * nb]
          linear(kb, ewd, KC, list(range(KC)), lambda k, c0, c1, aT=aT, aTt=aTt: (aT[:, k, c0:c1], [aTt]), blocks, epi_dn, wpool, banks)
          kb.release(m0)
    return kb.finish()


def build_l1d():
    kb = KB()
    x2T = kb.din("x2T", [D, TL])
    ya = kb.din("ya", [KC, 128, TL])
    yb = kb.din("yb", [KC, 128, TL])
    outT = kb.dout("outT", [D, TL])
    pa = Pool(kb, 'da', 2, TL, F32)
    pb = Pool(kb, 'db', 2, TL, F32)
    px = Pool(kb, 'dx', 2, TL, F32)
    for n in range(KC):
        xb, xt, xn = px.next()
        ab, at, an = pa.next()
        bb, bt, bn = pb.next()
        kb.dma('sync', xb, x2T[n * 128:(n + 1) * 128, :], [], [xt], "L" + xn)
        kb.dma('sync', ab, ya[n], [], [at], "L" + an)
        kb.dma('sync', bb, yb[n], [], [bt], "L" + bn)
        kb.I('vector', 'tensor_tensor', [at, bt], [at], ab, ab, bb, ALU.add)
        kb.I('vector', 'tensor_tensor', [at, xt], [xt], xb, xb, ab, ALU.add)
        kb.dma('sync', outT[n * 128:(n + 1) * 128, :], xb, [xt], [], "S" + xn)
    return kb.finish()


def host_moe(r1b, g2_lat, ewg, ewu, ewd):
    NT = TL // 128
    NS = 2
    def tokmajor(a):
        return a.reshape(128, NT, NE).transpose(1, 0, 2).reshape(TL, NE)
    comb = np.concatenate([tokmajor(r1b[i]["comb"]) for i in range(NCORES)], axis=0)
    mask = np.concatenate([tokmajor(r1b[i]["mask"]) for i in range(NCORES)], axis=0) > 0.5
    h3 = np.concatenate([r1b[i]["h3T"].reshape(D, TL) for i in range(NCORES)], axis=1)
    idx = [np.nonzero(mask[:, e])[0] for e in range(NE)]
    counts = [len(ix) for ix in idx]
    Cs = 1024
    while sum(-(-n // Cs) for n in counts) > NS * NCORES:
        Cs += 1024
    items = []
    for e in range(NE):
        for s0 in range(0, counts[e], Cs):
            items.append((e, s0, min(counts[e], s0 + Cs)))
    while len(items) < NS * NCORES:
        items.append((0, 0, 0))
    in_maps = [{"g2": g2_lat} for _ in range(NCORES)]
    for j, (e, s0, s1) in enumerate(items):
        core, slot = j % NCORES, j // NCORES
        n = s1 - s0
        tok = idx[e][s0:s1]
        hg = np.zeros((D, Cs), NPBF)
        hg[:, :n] = h3[:, tok]
        wr = np.zeros((128, Cs), np.float32)
        wr[:, :n] = comb[tok, e][None, :]
        m = in_maps[core]
        m[f"hg{slot}"] = hg.reshape(KC, 128, Cs)
        m[f"wrow{slot}"] = wr
        m[f"ewg{slot}"] = np.ascontiguousarray(ewg[e])
        m[f"ewu{slot}"] = np.ascontiguousarray(ewu[e])
        m[f"ewd{slot}"] = np.ascontiguousarray(ewd[e])
    rc = run(f"l1c_{NS}_{Cs}", lambda: build_l1c(NS, Cs), in_maps)
    rank = np.cumsum(mask, axis=1) - 1
    Ya = np.zeros((D, S), np.float32)
    Yb = np.zeros((D, S), np.float32)
    for j, (e, s0, s1) in enumerate(items):
        core, slot = j % NCORES, j // NCORES
        n = s1 - s0
        if n == 0:
            continue
        tok = idx[e][s0:s1]
        y = rc[core][f"yT{slot}"].reshape(D, Cs)[:, :n]
        sl = rank[tok, e]
        Ya[:, tok[sl == 0]] = y[:, sl == 0]
        Yb[:, tok[sl >= 1]] = y[:, sl >= 1]
    in_maps = []
    for i in range(NCORES):
        in_maps.append({"x2T": np.ascontiguousarray(r1b[i]["outT"]),
                        "ya": np.ascontiguousarray(Ya[:, i * TL:(i + 1) * TL]).reshape(KC, 128, TL),
                        "yb": np.ascontiguousarray(Yb[:, i * TL:(i + 1) * TL]).reshape(KC, 128, TL)})
    return run("l1d", build_l1d, in_maps)


def kernel(**I):
    g = lambda k: np.asarray(I[k])
    mod = host_mod(g('c'), g('c_ctx'), g('attn_w_mod')[0], g('attn_b_mod')[0], g('rec_w_mod')[0], g('rec_b_mod')[0])
    x = g('x')[0]
    ctx = g('ctx')[0]
    r0a = host_l0a(x, ctx, mod[0], g('attn_norm_mix')[0], g('attn_w_in')[0], g('attn_gqa_q_norm')[0], g('attn_gqa_k_norm')[0],
                   g('attn_diff_q_norm')[0], g('attn_diff_k_norm')[0])
    r0b1 = host_l0b1(r0a, g('attn_diff_lambda_q1')[0], g('attn_diff_lambda_k1')[0], g('attn_diff_lambda_q2')[0],
                     g('attn_diff_lambda_k2')[0], g('attn_diff_subln')[0])
    del r0a
    r0b2 = host_l0b2(r0b1, x, ctx, mod[0], g('attn_norm_ffn')[0], g('attn_w_out')[0], g('ffn_w_gate')[0], g('ffn_w_up')[0],
                     g('ffn_w_down')[0])
    del r0b1
    x1 = np.ascontiguousarray(np.concatenate([r0b2[i]["x1T"][:, 0:TL] for i in range(NCORES)], axis=1).T)
    ctx1 = np.ascontiguousarray(r0b2[0]["x1T"][:, TL:T0].T)
    del r0b2
    r1a = host_l1a(x1, ctx1, mod[1], g('rec_norm_mix')[0], g('rec_w_in')[0], g('rec_conv_w')[0], g('rec_conv_b')[0],
                   g('rec_gate_a_w')[0], g('rec_gate_a_b')[0], g('rec_gate_x_w')[0], g('rec_gate_x_b')[0], g('rec_lru_lambda')[0])
    r1b = host_l1b(r1a, x1, mod[1], g('rec_norm_ffn')[0], g('rec_w_out')[0], g('moe_router_w')[0], g('moe_router_b')[0])
    del r1a
    g2_lat = fm(mod[1][0, 5])
    r1d = host_moe(r1b, g2_lat, g('moe_w_gate')[0], g('moe_w_up')[0], g('moe_w_down')[0])
    out = np.concatenate([r1d[i]["outT"] for i in range(NCORES)], axis=1).T
    return np.ascontiguousarray(out.reshape(1, S, D).astype(np.float32))
```
